# Optimizing a Trainium2 kernel written in Bass

```python
import jax, jax.numpy as jnp
from jax import lax
import numpy as np

D_MODEL = 1024
BATCH = 32
SEQ = 2048
DEPTH = 1

N_MEM = 256
MIX_WIDTH = D_MODEL
POOL_WIDTH = MIX_WIDTH // 2
POOL_GROUPS = 4
POOL_GROUP_DIM = POOL_WIDTH // POOL_GROUPS
POOL_WINDOWS = (2, 4, 8, 16)
GLA_VALUE_WIDTH = MIX_WIDTH - POOL_WIDTH
GLA_KEY_WIDTH = GLA_VALUE_WIDTH // 2
GLA_HEADS = 4
GLA_DK = GLA_KEY_WIDTH // GLA_HEADS
GLA_DV = GLA_VALUE_WIDTH // GLA_HEADS
GLA_GATE_RANK = 16
GLA_GATE_NORMALIZER = 16.0
GLA_CHUNK = 64
OFF_Q = POOL_WIDTH
OFF_K = OFF_Q + GLA_KEY_WIDTH
OFF_V = OFF_K + GLA_KEY_WIDTH
OFF_G = OFF_V + GLA_VALUE_WIDTH
OFF_R = OFF_G + GLA_GATE_RANK
IN_PROJ_WIDTH = OFF_R + GLA_VALUE_WIDTH
XATTN_HEADS = 4
XATTN_HEAD_DIM = D_MODEL // XATTN_HEADS
D_FF = 2816
CONV_WIDTH = 3
EPS = 1e-6

kernel_name = 'hybrid_pool_gla_memxattn_convffn'


def rms_norm(x, w):
    x32 = x.astype(jnp.float32)
    y = x32 * lax.rsqrt(jnp.mean(x32 * x32, axis=-1, keepdims=True) + EPS)
    return (y * w.astype(jnp.float32)).astype(x.dtype)


def multiscale_pool(p, pool_w, pool_scale):
    b, s, _ = p.shape
    pg = p.reshape(b, s, POOL_GROUPS, POOL_GROUP_DIM)
    cs = jnp.cumsum(pg.astype(jnp.float32), axis=1)
    t = jnp.arange(s)
    pooled = []
    for g, w in enumerate(POOL_WINDOWS):
        c = cs[:, :, g]
        c_prev = jnp.pad(c, ((0, 0), (w, 0), (0, 0)))[:, :s]
        cnt = jnp.minimum(t + 1, w).astype(jnp.float32)[None, :, None]
        pooled.append((c - c_prev) / cnt)
    pooled = jnp.stack(pooled, axis=2).astype(p.dtype) - pg
    mixed = jnp.einsum('bsgc,gcd->bsgd', pooled, pool_w)
    return mixed.reshape(b, s, POOL_WIDTH) * pool_scale


def gla_chunked(q, k, v, log_g):
    b, h, s, dk = q.shape
    dv = v.shape[-1]
    nc = s // GLA_CHUNK
    f32 = jnp.float32
    q = q.astype(f32).reshape(b, h, nc, GLA_CHUNK, dk) * (dk ** -0.5)
    k = k.astype(f32).reshape(b, h, nc, GLA_CHUNK, dk)
    v = v.astype(f32).reshape(b, h, nc, GLA_CHUNK, dv)
    G = jnp.cumsum(log_g.astype(f32).reshape(b, h, nc, GLA_CHUNK, dk), axis=3)
    G_last = G[:, :, :, -1]
    q_dec = q * jnp.exp(G)
    k_dec = k * jnp.exp(-G)
    causal = jnp.tril(jnp.ones((GLA_CHUNK, GLA_CHUNK), dtype=bool))
    scores = jnp.where(causal, jnp.einsum('bhnid,bhnjd->bhnij', q_dec, k_dec), 0.0)
    o_intra = jnp.einsum('bhnij,bhnje->bhnie', scores, v)
    k_to_end = k * jnp.exp(G_last[:, :, :, None] - G)
    chunk_kv = jnp.einsum('bhnjd,bhnje->bhnde', k_to_end, v)
    decay = jnp.exp(G_last)

    def step(state, inp):
        dec, kv = inp
        return state * dec[..., None] + kv, state

    init = jnp.zeros((b, h, dk, dv), f32)
    _, prev_states = lax.scan(step, init, (jnp.moveaxis(decay, 2, 0), jnp.moveaxis(chunk_kv, 2, 0)))
    prev_states = jnp.moveaxis(prev_states, 0, 2)
    o_inter = jnp.einsum('bhnid,bhnde->bhnie', q_dec, prev_states)
    return (o_intra + o_inter).reshape(b, h, s, dv)


def hybrid_mixer(h, w_in, pool_w, pool_scale, gk_w2, gk_b, gla_norm_w, w_out):
    b, s, _ = h.shape
    proj = h @ w_in
    p = proj[..., :OFF_Q]
    q = proj[..., OFF_Q:OFF_K]
    k = proj[..., OFF_K:OFF_V]
    v = proj[..., OFF_V:OFF_G]
    g_low = proj[..., OFF_G:OFF_R]
    r = proj[..., OFF_R:]
    pool_out = multiscale_pool(p, pool_w, pool_scale)
    log_g = jax.nn.log_sigmoid((g_low @ gk_w2 + gk_b).astype(jnp.float32)) / GLA_GATE_NORMALIZER

    def heads(t, d):
        return t.reshape(b, s, GLA_HEADS, d).transpose(0, 2, 1, 3)

    o = gla_chunked(heads(q, GLA_DK), heads(k, GLA_DK), heads(v, GLA_DV), heads(log_g, GLA_DK))
    o = rms_norm(o, gla_norm_w).transpose(0, 2, 1, 3).reshape(b, s, GLA_VALUE_WIDTH).astype(h.dtype)
    gla_out = o * jax.nn.silu(r)
    return jnp.concatenate([pool_out, gla_out], axis=-1) @ w_out


def memory_cross_attention(h, mem_n, wq, wkv, wo):
    b, s, _ = h.shape
    m = mem_n.shape[1]
    q = (h @ wq).reshape(b, s, XATTN_HEADS, XATTN_HEAD_DIM)
    kv = mem_n @ wkv
    k = kv[..., :D_MODEL].reshape(b, m, XATTN_HEADS, XATTN_HEAD_DIM)
    v = kv[..., D_MODEL:].reshape(b, m, XATTN_HEADS, XATTN_HEAD_DIM)
    scores = jnp.einsum('bshd,bmhd->bhsm', q, k).astype(jnp.float32) * (XATTN_HEAD_DIM ** -0.5)
    probs = jax.nn.softmax(scores, axis=-1).astype(v.dtype)
    o = jnp.einsum('bhsm,bmhd->bshd', probs, v).reshape(b, s, D_MODEL)
    return o @ wo


def conv_ffn(h, w_up, conv_w, conv_b, w_down):
    s = h.shape[1]
    u = h @ w_up
    u_pad = jnp.pad(u, ((0, 0), (CONV_WIDTH - 1, 0), (0, 0)))
    u = conv_b + conv_w[0] * u_pad[:, 0:s] + conv_w[1] * u_pad[:, 1:1 + s] + conv_w[2] * u_pad[:, 2:2 + s]
    gate = u[..., :D_FF]
    val = u[..., D_FF:]
    return (jax.nn.silu(gate) * val) @ w_down


def setup_inputs(seed: int = 0) -> dict:
    key = jax.random.key(seed)
    ks = jax.random.split(key, 24)
    f32 = jnp.float32
    L = DEPTH

    def nrm(k, shape, scale):
        return jax.random.normal(k, shape, f32) * scale

    def gain(k, shape):
        return 1.0 + 0.05 * jax.random.normal(k, shape, f32)

    return {
        'x': jax.random.normal(ks[0], (BATCH, SEQ, D_MODEL), f32),
        'mem': jax.random.normal(ks[1], (BATCH, N_MEM, D_MODEL), f32),
        'norm_mix_w': gain(ks[2], (L, D_MODEL)),
        'w_in': nrm(ks[3], (L, D_MODEL, IN_PROJ_WIDTH), D_MODEL ** -0.5),
        'pool_w': nrm(ks[4], (L, POOL_GROUPS, POOL_GROUP_DIM, POOL_GROUP_DIM), POOL_GROUP_DIM ** -0.5),
        'pool_scale': 0.5 + 0.05 * jax.random.normal(ks[5], (L, POOL_WIDTH), f32),
        'gk_w2': nrm(ks[6], (L, GLA_GATE_RANK, GLA_KEY_WIDTH), GLA_GATE_RANK ** -0.5),
        'gk_b': nrm(ks[7], (L, GLA_KEY_WIDTH), 0.1),
        'gla_norm_w': gain(ks[8], (L, GLA_DV)),
        'w_out': nrm(ks[9], (L, MIX_WIDTH, D_MODEL), MIX_WIDTH ** -0.5),
        'norm_xattn_w': gain(ks[10], (L, D_MODEL)),
        'norm_mem_w': gain(ks[11], (L, D_MODEL)),
        'xattn_wq': nrm(ks[12], (L, D_MODEL, D_MODEL), D_MODEL ** -0.5),
        'xattn_wkv': nrm(ks[13], (L, D_MODEL, 2 * D_MODEL), D_MODEL ** -0.5),
        'xattn_wo': nrm(ks[14], (L, D_MODEL, D_MODEL), D_MODEL ** -0.5),
        'norm_ffn_w': gain(ks[15], (L, D_MODEL)),
        'ffn_w_up': nrm(ks[16], (L, D_MODEL, 2 * D_FF), D_MODEL ** -0.5),
        'ffn_conv_w': nrm(ks[17], (L, CONV_WIDTH, 2 * D_FF), CONV_WIDTH ** -0.5),
        'ffn_conv_b': nrm(ks[18], (L, 2 * D_FF), 0.01),
        'ffn_w_down': nrm(ks[19], (L, D_FF, D_MODEL), D_FF ** -0.5),
        'norm_final_w': gain(ks[20], (D_MODEL,)),
    }


def reference(x, mem, norm_mix_w, w_in, pool_w, pool_scale, gk_w2, gk_b, gla_norm_w, w_out,
              norm_xattn_w, norm_mem_w, xattn_wq, xattn_wkv, xattn_wo,
              norm_ffn_w, ffn_w_up, ffn_conv_w, ffn_conv_b, ffn_w_down, norm_final_w):
    for l in range(DEPTH):
        h = rms_norm(x, norm_mix_w[l])
        x = x + hybrid_mixer(h, w_in[l], pool_w[l], pool_scale[l], gk_w2[l], gk_b[l], gla_norm_w[l], w_out[l])
        h = rms_norm(x, norm_xattn_w[l])
        m = rms_norm(mem, norm_mem_w[l])
        x = x + memory_cross_attention(h, m, xattn_wq[l], xattn_wkv[l], xattn_wo[l])
        h = rms_norm(x, norm_ffn_w[l])
        x = x + conv_ffn(h, ffn_w_up[l], ffn_conv_w[l], ffn_conv_b[l], ffn_w_down[l])
    return rms_norm(x, norm_final_w)
```

```python
import contextlib
import numpy as np
import concourse.bass as bass
import concourse.mybir as mybir
from concourse.bass_utils import run_bass_kernel_spmd

F32 = mybir.dt.float32
BF16 = mybir.dt.bfloat16
AF = mybir.ActivationFunctionType
ALU = mybir.AluOpType

D = 1024
SEQ = 2048
NMEM = 256
T = 512
NB = 4
KC = 8
DFF = 2816
NCH = 44
EPS = 1e-6
NCORES = 8
SEQ_PER_CORE = 4
RING = 4
NCV = 8
STRICT = True


class Sched:
    ENGS = ("pe", "act", "dve", "pool", "sp")

    def __init__(self, nc):
        self.nc = nc
        self.ops = {e: [] for e in self.ENGS}
        self.cnt = {}
        self.seen = {e: {} for e in self.ENGS}
        self.lastw = {}
        self.readers = {}
        self.chan_last = {}
        self.mute = False

    def _deps(self, reads, writes, eng=None):
        deps = []
        own = ("c", eng)
        for b in reads:
            t = self.lastw.get(b)
            if t is not None:
                deps.append(t)
            if isinstance(b, tuple) and b[0] in ("ps", "pt"):
                r = self.readers.get(b)
                if r:
                    deps.extend((k, v) for k, v in r.items() if k != own)
        for b in writes:
            t = self.lastw.get(b)
            if t is not None and (STRICT or t[0] != own):
                deps.append(t)
            r = self.readers.get(b)
            if r:
                deps.extend((k, v) for k, v in r.items() if (STRICT or k != own))
        return deps

    def _commit(self, tok, reads, writes):
        k, v = tok
        for b in reads:
            d = self.readers.setdefault(b, {})
            if d.get(k, 0) < v:
                d[k] = v
        for b in writes:
            self.lastw[b] = tok
            self.readers[b] = {}

    def _waits(self, eng, deps):
        need = {}
        for (k, v) in deps:
            if eng == "pe" and k == ("c", "pe"):
                continue
            if need.get(k, 0) < v:
                need[k] = v
        out = []
        seen = self.seen[eng]
        for k, v in need.items():
            if seen.get(k, 0) < v:
                seen[k] = v
                out.append((k, v))
        return out

    def op(self, eng, fn, reads=(), writes=()):
        if self.mute:
            return None
        waits = self._waits(eng, self._deps(reads, writes, eng))
        k = ("c", eng)
        self.cnt[k] = self.cnt.get(k, 0) + 1
        tok = (k, self.cnt[k])
        self._commit(tok, reads, writes)
        self.ops[eng].append((waits, fn, k, 1))
        return tok

    def dma(self, eng, chan, fn, reads=(), writes=()):
        if self.mute:
            return None
        deps = self._deps(reads, writes)
        k = ("d", chan)
        if k in self.chan_last:
            deps.append(self.chan_last[k])
        waits = self._waits(eng, deps)
        self.cnt[k] = self.cnt.get(k, 0) + 16
        tok = (k, self.cnt[k])
        self.chan_last[k] = tok
        self._commit(tok, reads, writes)
        self.ops[eng].append((waits, fn, k, 16))
        return tok

    def wait_all(self, eng, toks):
        waits = self._waits(eng, [t for t in toks if t is not None])
        self.ops[eng].append((waits, None, None, 0))

    def emit(self):
        nc = self.nc
        sems = {}
        with contextlib.ExitStack() as st:
            for k in self.cnt:
                sems[k] = st.enter_context(nc.semaphore("s_%s_%s" % k))
            block = st.enter_context(nc.Block())

            def run(engobj, e):
                for (waits, fn, semk, incv) in self.ops[e]:
                    for (k, v) in waits:
                        engobj.wait_ge(sems[k], v)
                    if fn is not None:
                        ins = fn(engobj)
                        ins.then_inc(sems[semk], incv)

            @block.tensor
            def _(eng):
                run(eng, "pe")

            @block.scalar
            def _(eng):
                run(eng, "act")

            @block.vector
            def _(eng):
                run(eng, "dve")

            @block.gpsimd
            def _(eng):
                run(eng, "pool")

            @block.sync
            def _(eng):
                run(eng, "sp")


def _chunk_col(q):
    j, r = divmod(q, 4)
    return (2 * j + (r % 2)) * 128 + (DFF if r >= 2 else 0)


def _panel(W, r0, nk, cols):
    P = np.zeros((128, KC, 512), np.float32)
    blk = W[r0:r0 + nk * 128][:, cols]
    P[:, :nk, :] = blk.reshape(nk, 128, 512).transpose(1, 0, 2)
    return P.reshape(128, KC * 512)


PANEL_NAMES = (["kv%d" % i for i in range(4)] + ["in_p", "in_qk", "in_v", "in_r", "wout0", "wout1",
               "wq0", "wq1", "wo0", "wo1"] + ["up%d" % i for i in range(11)] +
               ["dn%d_%d" % (nh, kg) for nh in range(2) for kg in range(3)])
PID = {n: i for i, n in enumerate(PANEL_NAMES)}
NPAN = len(PANEL_NAMES)
TILE_PANELS = (["in_p", "in_qk", "in_v", "in_r", "wout0", "wout1", "wq0", "wq1", "wo0", "wo1"] +
               ["up%d" % i for i in range(11)] + ["dn%d_%d" % (nh, kg) for nh in range(2) for kg in range(3)])


def _pack_weights(w_in, w_out, wq, wkv, wo, w_up, w_down):
    ar = np.arange(512)
    out = np.zeros((NPAN, 128, KC * 512), np.float32)
    for i in range(4):
        out[PID["kv%d" % i]] = _panel(wkv, 0, 8, ar + 512 * i)
    out[PID["in_p"]] = _panel(w_in, 0, 8, ar)
    out[PID["in_qk"]] = _panel(w_in, 0, 8, ar + 512)
    out[PID["in_v"]] = _panel(w_in, 0, 8, ar + 1024)
    out[PID["in_r"]] = _panel(w_in, 0, 8, ar + 1552)
    for i in range(2):
        out[PID["wout%d" % i]] = _panel(w_out, 0, 8, ar + 512 * i)
        out[PID["wq%d" % i]] = _panel(wq, 0, 8, ar + 512 * i)
        out[PID["wo%d" % i]] = _panel(wo, 0, 8, ar + 512 * i)
    for j in range(11):
        cols = np.concatenate([np.arange(128) + _chunk_col(4 * j + r) for r in range(4)])
        out[PID["up%d" % j]] = _panel(w_up, 0, 8, cols)
    for nh in range(2):
        for kg in range(3):
            nk = 8 if kg < 2 else 6
            out[PID["dn%d_%d" % (nh, kg)]] = _panel(w_down, kg * 1024, nk, ar + 512 * nh)
    return out


def _consts():
    s = np.arange(128)[:, None]
    t = np.arange(128)[None, :]
    same = (s // 64) == (t // 64)
    tri = np.where(same & (s <= t), -1.0 / 16.0, 0.0).astype(np.float32)
    tris = np.where(same & (s > t), -1.0 / 16.0, 0.0).astype(np.float32)
    j = (np.arange(128) % 64)[:, None, None]
    i = np.arange(64)[None, None, :]
    cmask = np.broadcast_to((j <= i), (128, 4, 64)).astype(np.float32).reshape(128, 256)
    ident = np.eye(128, dtype=np.float32)
    invc = np.zeros((128, 4, 16), np.float32)
    tt = np.arange(16)
    for g, w in enumerate((2, 4, 8, 16)):
        invc[:, g, :] = 1.0 / np.minimum(tt + 1, w)
    return tri, tris, cmask, ident, invc.reshape(128, 64)


def build_program(nseq=SEQ_PER_CORE, ntile=SEQ // T, dbg=None):
    nc = bass.Bass("TRN2", target_bir_lowering=False)
    S = Sched(nc)
    en = dbg
    import os as _os
    MIXSTOP = int(_os.environ.get("MIXSTOP", "99"))

    def ck(n):
        if n > MIXSTOP:
            S.mute = True

    def din(name, shape):
        return nc.dram_tensor(name, list(shape), F32, kind="ExternalInput")

    x_d = din("x", [nseq * SEQ, D])
    mem_d = din("mem", [nseq * NMEM, D])
    wpan_d = din("wpan", [NPAN, 128, KC * 512])
    wg_d = din("wg", [128, KC * 16])
    nw_d = din("nw", [128, 4 * KC])
    wf_d = din("wf", [1, D])
    poolw_d = din("poolw", [128, 4 * 128])
    pscale_d = din("pscale", [128, 4])
    gkw_d = din("gkw", [17, 256])
    gnw_d = din("gnw", [128, 1])
    cw_d = din("cw", [128, NCH * 3])
    cb_d = din("cb", [128, NCH])
    tri_d = din("tri", [128, 128])
    tris_d = din("tris", [128, 128])
    cmask_d = din("cmask", [128, 256])
    ident_d = din("ident", [128, 128])
    invc_d = din("invc", [128, 64])
    out_d = nc.dram_tensor("out", [nseq * SEQ, D], F32, kind="ExternalOutput")
    wbf_d = nc.dram_tensor("wbf", [NPAN, 128, KC * 512], BF16, kind="Internal")

    def sb(name, shape, dt=F32):
        return nc.alloc_sbuf_tensor("s_" + name, list(shape), dt)

    xbuf = [sb("xb%d" % i, [128, NB, D]) for i in range(2)]
    ring = [sb("ring%d" % i, [128, KC, 512], BF16) for i in range(RING)]
    hT = sb("hT", [128, KC, T], BF16)
    hb = sb("hb", [128, NB, D], BF16)
    junk = sb("junk", [128, D], BF16)
    ss = sb("ss", [128, 4])
    rstd = sb("rstd", [128, 4])
    epsc = sb("epsc", [128, 1])
    wf_bc = sb("wf_bc", [128, D])
    nw = sb("nw", [128, 4, KC])
    wg_f = sb("wg_f", [128, KC, 16])
    wg_b = sb("wg_b", [128, KC, 16], BF16)
    poolw_f = sb("poolw_f", [128, 4, 128])
    poolw_b = sb("poolw_b", [128, 4, 128], BF16)
    pscale = sb("pscale", [128, 4])
    gkw = sb("gkw", [17, 256])
    gnw = sb("gnw", [128, 1])
    cw = sb("cw", [128, NCH, 3])
    cb = sb("cb", [128, NCH])
    tri = sb("tri", [128, 128])
    tris = sb("tris", [128, 128])
    cmask = sb("cmask", [128, 4, 64])
    cmask2 = cmask[:].rearrange("p (a b) c -> p a (b c)", a=2)
    ident_f = sb("ident_f", [128, 128])
    ident_b = sb("ident_b", [128, 128], BF16)
    invc = sb("invc", [128, 4, 16])
    ones_f = sb("ones_f", [128, 128])
    ones_b = sb("ones_b", [128, 128], BF16)
    KT = sb("KT", [128, 8, NMEM], BF16)
    Vm = sb("Vm", [128, 2, D], BF16)
    pext = sb("pext", [128, 4, 16 + T])
    Sst = sb("Sst", [128, 2, 128])
    halo = sb("halo", [128, NCH, 2])
    dec = sb("dec", [128, 2, 8])
    gT = sb("gT", [17, T])
    m_sb = sb("m_sb", [128, 2, D])

    ARENA_KB = 80
    arena = sb("arena", [128, ARENA_KB * 256])

    class AV:
        def __init__(self, off_kb, shape, dt):
            esz = 4 if dt == F32 else 2
            n = int(np.prod(shape))
            self.off = off_kb * 1024
            self.esz = esz
            self.n = n
            assert self.off + n * esz <= ARENA_KB * 1024, (off_kb, shape)
            a = arena[:, self.off // 4:(self.off + n * esz + 3) // 4]
            if dt != F32:
                a = a.bitcast(dt)
            if len(shape) == 2:
                a = a.rearrange("p (a b) -> p a b", a=shape[0])
            elif len(shape) == 3:
                a = a.rearrange("p (a b c) -> p a b c", a=shape[0], b=shape[1])
            self.ap = a
            self.shape = shape

        def k(self, lo=0, hi=None):
            hi = self.n if hi is None else hi
            b0 = (self.off + lo * self.esz) // 1024
            b1 = (self.off + hi * self.esz - 1) // 1024
            return [("ar", i) for i in range(b0, b1 + 1)]

        def kc(self, i, j=None):
            per = self.n // self.shape[0]
            j = i + 1 if j is None else j
            return self.k(i * per, j * per)

    catT = AV(0, [8, T], BF16)
    o_sb = AV(8, [4, T], F32)
    sq = AV(16, [4, T], F32)
    rb = [AV(24 + 2 * i, [T], F32) for i in range(2)]
    gl = AV(28, [T], F32)
    lg = AV(30, [4, 256], F32)
    eD = AV(34, [4, 256], F32)
    eG = AV(38, [2, T], F32)
    enG = AV(42, [2, T], F32)
    qdec = AV(46, [2, T], BF16)
    kdec = AV(48, [2, T], BF16)
    kte = AV(50, [4, 256], BF16)
    vtok = AV(52, [4, T], BF16)
    scm = AV(56, [4, 256], BF16)
    sr = AV(58, [4, T], F32)
    pooled = AV(66, [4, T], BF16)
    pa = [AV(70 + 3 * i, [16 + T], F32) for i in range(2)]
    Sbf = AV(76, [8, 256], BF16)
    oxT = AV(0, [8, T], BF16)
    qxT = AV(8, [8, T], BF16)
    PT = AV(16, [8, T], BF16)
    rs = [AV(24 + 2 * i, [T], F32) for i in range(2)]
    actT = AV(8, [22, T], BF16)
    ubuf = [AV(30 + 3 * i, [2 + T], F32) for i in range(4)]
    ybuf = [AV(42 + 2 * i, [T], F32) for i in range(4)]
    sg = [AV(50 + 2 * i, [T], F32) for i in range(2)]

    NPS = 6
    psb = [nc.alloc_psum_tensor("ps%d" % i, [128, 512], F32) for i in range(NPS)]
    ptb = [nc.alloc_psum_tensor("pt%d" % i, [128, 512], BF16) for i in range(2)]
    rr = {"ps": 0, "pt": 0}

    def nb():
        i = rr["ps"]
        rr["ps"] = (i + 1) % NPS
        return psb[i], ("ps", i)

    def npt():
        i = rr["pt"]
        rr["pt"] = (i + 1) % 2
        return ptb[i], ("pt", i)

    def act(fn, reads, writes):
        return S.op("act", fn, reads, writes)

    def dve(fn, reads, writes):
        return S.op("dve", fn, reads, writes)

    def pool(fn, reads, writes):
        return S.op("pool", fn, reads, writes)

    def mm(out_ap, pairs, reads, writes):
        n = len(pairs)

        def fn(e):
            ins = None
            for i, (l, r) in enumerate(pairs):
                ins = e.matmul(out_ap, l, r, start=(i == 0), stop=(i == n - 1))
            return ins
        return S.op("pe", fn, reads, writes)

    def mm_multi(groups, reads, writes):
        def fn(e):
            ins = None
            for out_ap, pairs in groups:
                n = len(pairs)
                for i, (l, r) in enumerate(pairs):
                    ins = e.matmul(out_ap, l, r, start=(i == 0), stop=(i == n - 1))
            return ins
        return S.op("pe", fn, reads, writes)

    cch = [0]

    def cload(dst_ap, src_ap, key):
        ch = "c%d" % (cch[0] % 4)
        cch[0] += 1
        S.dma("sp", ch, lambda e: e.dma_start(out=dst_ap, in_=src_ap), writes=[key])

    cload(nw[:], nw_d.ap().rearrange("p (a b) -> p a b", a=4), "nw")
    cload(wg_f[:], wg_d.ap().rearrange("p (a b) -> p a b", a=KC), "wg_f")
    cload(wf_bc[:], wf_d.ap().partition_broadcast(128), "wf_bc")
    cload(poolw_f[:], poolw_d.ap().rearrange("p (a b) -> p a b", a=4), "poolw_f")
    cload(pscale[:], pscale_d.ap(), "pscale")
    cload(gkw[:], gkw_d.ap(), "gkw")
    cload(gnw[:], gnw_d.ap(), "gnw")
    cload(cw[:], cw_d.ap().rearrange("p (a b) -> p a b", a=NCH), "cw")
    cload(cb[:], cb_d.ap(), "cb")
    cload(tri[:], tri_d.ap(), "tri")
    cload(tris[:], tris_d.ap(), "tris")
    cload(cmask[:], cmask_d.ap().rearrange("p (a b) -> p a b", a=4), "cmask")
    cload(ident_f[:], ident_d.ap(), "ident_f")
    cload(invc[:], invc_d.ap().rearrange("p (a b) -> p a b", a=4), "invc")
    dve(lambda e: e.tensor_copy(out=ident_b[:], in_=ident_f[:]), ["ident_f"], ["ident_b"])
    dve(lambda e: e.tensor_copy(out=wg_b[:], in_=wg_f[:]), ["wg_f"], ["wg_b"])
    dve(lambda e: e.tensor_copy(out=poolw_b[:], in_=poolw_f[:]), ["poolw_f"], ["poolw_b"])
    dve(lambda e: e.memset(ones_f[:], 1.0 / 128.0), [], ["ones_f"])
    dve(lambda e: e.memset(ones_b[:], 1.0), [], ["ones_b"])
    dve(lambda e: e.memset(epsc[:], EPS), [], ["epsc"])
    dve(lambda e: e.memset(gT[:], 1.0), [], ["gT"])

    for j in range(NPAN):
        S.dma("pool", "cv%d" % (j % NCV),
              lambda e, j=j: e.dma_start(out=wbf_d.ap()[j], in_=wpan_d.ap()[j]),
              writes=[("wbf", j)])

    seq_panels = []
    for s in range(nseq):
        seq_panels += ["kv%d" % i for i in range(4)]
        for t in range(ntile):
            seq_panels += TILE_PANELS
    pstate = {"next_load": 0, "next_use": 0}

    def panel_load():
        i = pstate["next_load"]
        if i >= len(seq_panels):
            return
        pstate["next_load"] = i + 1
        slot = i % RING
        pid = PID[seq_panels[i]]
        m_ = S.mute
        S.mute = False
        S.dma("sp", "ring%d" % slot,
              lambda e: e.dma_start(out=ring[slot][:].rearrange("p a b -> p (a b)"), in_=wbf_d.ap()[pid]),
              reads=[("wbf", pid)], writes=[("ring", slot)])
        S.mute = m_

    def panel_next(expect):
        i = pstate["next_use"]
        assert seq_panels[i] == expect, (seq_panels[i], expect)
        pstate["next_use"] = i + 1
        slot = i % RING
        return ring[slot], ("ring", slot)

    for _ in range(RING):
        panel_load()

    def norm_T(xap_of_block, xkeys_of_block, nblk, widx):
        ntok = nblk * 128
        for b in range(nblk):
            xin = xap_of_block(b)
            act(lambda e, b=b, xin=xin: e.activation(out=junk[:], in_=xin, func=AF.Square,
                                                     accum_out=ss[:, b:b + 1]),
                xkeys_of_block(b), ["junk", ("ss", b)])
        sskeys = [("ss", b) for b in range(nblk)]
        act(lambda e: e.activation(out=rstd[:, 0:nblk], in_=ss[:, 0:nblk], func=AF.Ln,
                                   scale=1.0 / D, bias=epsc[:, 0:1]), sskeys + ["epsc"], ["rstd"])
        act(lambda e: e.activation(out=rstd[:, 0:nblk], in_=rstd[:, 0:nblk], func=AF.Exp, scale=-0.5),
            ["rstd"], ["rstd"])
        for b in range(nblk):
            xin = xap_of_block(b)
            dve(lambda e, b=b, xin=xin: e.tensor_scalar(out=hb[:, b, :], in0=xin,
                                                        scalar1=rstd[:, b:b + 1], scalar2=None, op0=ALU.mult),
                xkeys_of_block(b) + ["rstd"], [("hb", b)])
        for kc in range(KC):
            pt, ptk = npt()

            def fn(e, kc=kc, pt=pt):
                ins = None
                for b in range(nblk):
                    ins = e.transpose(out=pt[:, b * 128:(b + 1) * 128],
                                      in_=hb[:, b, kc * 128:(kc + 1) * 128], identity=ident_b[:])
                return ins
            S.op("pe", fn, [("hb", b) for b in range(nblk)] + ["ident_b"], [ptk])
            if kc % 2 == 0:
                act(lambda e, kc=kc, pt=pt: e.activation(out=hT[:, kc, 0:ntok], in_=pt[:, 0:ntok], func=AF.Copy,
                                                         scale=nw[:, widx, kc:kc + 1]),
                    [ptk, "nw"], [("hT", kc)])
            else:
                dve(lambda e, kc=kc, pt=pt: e.tensor_scalar(out=hT[:, kc, 0:ntok], in0=pt[:, 0:ntok],
                                                            scalar1=nw[:, widx, kc:kc + 1], scalar2=None,
                                                            op0=ALU.mult),
                    [ptk, "nw"], [("hT", kc)])

    hTk = [("hT", kc) for kc in range(KC)]

    def proj_residual(src, names, xb, xk):
        for nh, nm in enumerate(names):
            pan, pk = panel_next(nm)
            for b in range(NB):
                bank, bk = nb()
                mm(bank[:, :], [(src.ap[:, kc, b * 128:(b + 1) * 128], pan[:, kc, :]) for kc in range(KC)],
                   src.k() + [pk], [bk])
                dve(lambda e, b=b, nh=nh, bank=bank: e.tensor_tensor(
                    out=xb[:, b, nh * 512:(nh + 1) * 512], in0=xb[:, b, nh * 512:(nh + 1) * 512],
                    in1=bank[:, :], op=ALU.add), [bk, (xk, b)], [(xk, b)])
            panel_load()

    def load_mem(s_):
        S.dma("sp", "mem", lambda e: e.dma_start(
            out=m_sb[:], in_=mem_d.ap()[s_ * NMEM:(s_ + 1) * NMEM, :].rearrange("(b p) d -> p b d", p=128)),
            writes=["m_sb"])

    def load_x(s_, t_):
        g = s_ * ntile + t_
        r0 = s_ * SEQ + t_ * T
        S.dma("sp", "x%d" % (g % 2), lambda e: e.dma_start(
            out=xbuf[g % 2][:], in_=x_d.ap()[r0:r0 + T, :].rearrange("(b p) d -> p b d", p=128)),
            writes=[("x%d" % (g % 2), b) for b in range(NB)])

    tile_idx = 0
    for s in range(nseq):
        if s == 0:
            load_mem(0)
            load_x(0, 0)
        S.mute = en is not None and "kv" not in en
        norm_T(lambda b: m_sb[:, b, :], lambda b: ["m_sb"], 2, 2)
        for i in range(2):
            pan, pk = panel_next("kv%d" % i)
            for c in range(4):
                bank, bk = nb()
                mm(bank[:, 0:NMEM], [(pan[:, kc, c * 128:(c + 1) * 128], hT[:, kc, 0:NMEM]) for kc in range(KC)],
                   hTk + [pk], [bk])
                act(lambda e, i=i, c=c, bank=bank: e.activation(out=KT[:, i * 4 + c, :], in_=bank[:, 0:NMEM],
                                                                func=AF.Copy), [bk], [("KT", i * 4 + c)])
            panel_load()
        for i in range(2):
            pan, pk = panel_next("kv%d" % (2 + i))
            for mb in range(2):
                bank, bk = nb()
                mm(bank[:, :], [(hT[:, kc, mb * 128:(mb + 1) * 128], pan[:, kc, :]) for kc in range(KC)],
                   hTk + [pk], [bk])
                act(lambda e, i=i, mb=mb, bank=bank: e.activation(out=Vm[:, mb, i * 512:(i + 1) * 512],
                                                                  in_=bank[:, :], func=AF.Copy),
                    [bk], [("Vm", mb)])
            panel_load()
        S.mute = False
        dve(lambda e: e.memset(Sst[:], 0.0), [], ["Sst"])
        dve(lambda e: e.memset(pext[:, :, 0:16], 0.0), [], ["pext_h"])
        dve(lambda e: e.memset(halo[:], 0.0), [], ["halo"] + [("halo", q) for q in range(NCH)])

        for t in range(ntile):
            xb = xbuf[tile_idx % 2]
            xk = "x%d" % (tile_idx % 2)
            row0 = s * SEQ + t * T
            xkeys = [(xk, b) for b in range(NB)]
            if t + 1 < ntile:
                load_x(s, t + 1)
            elif s + 1 < nseq:
                load_x(s + 1, 0)
                load_mem(s + 1)

            S.mute = en is not None and "mix" not in en
            norm_T(lambda b: xb[:, b, :], lambda b: [(xk, b)], NB, 0)
            bank, bk = nb()
            mm(bank[0:16, :], [(wg_b[:, kc, :], hT[:, kc, :]) for kc in range(KC)], hTk + ["wg_b"], [bk])
            act(lambda e, bank=bank: e.activation(out=gT[0:16, :], in_=bank[0:16, :], func=AF.Copy), [bk], ["gT"])
            for b in range(NB):
                if b % 2 == 0:
                    zb, zk = nb()
                zo = zb[:, (b % 2) * 256:(b % 2 + 1) * 256]
                mm(zo, [(gT[0:17, b * 128:(b + 1) * 128], gkw[0:17, :])], ["gT", "gkw"], [zk])
                act(lambda e, b=b, zo=zo: e.activation(out=lg.ap[:, b, :], in_=zo, func=AF.Exp, scale=-1.0),
                    [zk], lg.kc(b))
                act(lambda e, b=b: e.activation(out=lg.ap[:, b, :], in_=lg.ap[:, b, :], func=AF.Ln, bias=1.0),
                    lg.kc(b), lg.kc(b))
            ck(1)
            pan, pk = panel_next("in_p")
            for c in range(4):
                bank, bk = nb()
                mm(bank[:, :], [(pan[:, kc, c * 128:(c + 1) * 128], hT[:, kc, :]) for kc in range(KC)],
                   hTk + [pk], [bk])
                act(lambda e, c=c, bank=bank: e.activation(out=pext[:, c, 16:16 + T], in_=bank[:, :], func=AF.Copy),
                    [bk], [("pext", c)])
            panel_load()
            ck(2)
            GT = []
            for pr in range(2):
                bank, bk = nb()
                mm_multi([(bank[:, b * 128:(b + 1) * 128], [(lg.ap[:, b, pr * 128:(pr + 1) * 128], tri[:, :])])
                          for b in range(NB)], lg.k() + ["tri"], [bk])
                GT.append((bank, bk))
                act(lambda e, pr=pr, bank=bank: e.activation(out=eG.ap[:, pr, :], in_=bank[:, :], func=AF.Exp),
                    [bk], eG.kc(pr))
                act(lambda e, pr=pr, bank=bank: e.activation(out=enG.ap[:, pr, :], in_=bank[:, :], func=AF.Exp,
                                                             scale=-1.0), [bk], enG.kc(pr))
                act(lambda e, pr=pr, bank=bank: e.activation(
                    out=dec[:, pr, :], in_=bank[:, :].rearrange("p (c j) -> p c j", j=64)[:, :, 63], func=AF.Exp),
                    [bk], [("dec", pr)])
            eDk = []
            for b in range(NB):
                if b % 2 == 0:
                    db, dk_ = nb()
                do = db[:, (b % 2) * 256:(b % 2 + 1) * 256]
                mm(do, [(tris[:, :], lg.ap[:, b, :])], lg.kc(b) + ["tris"], [dk_])
                act(lambda e, do=do, b=b: e.activation(out=eD.ap[:, b, :], in_=do, func=AF.Exp), [dk_], eD.kc(b))
            ck(3)
            pan, pk = panel_next("in_qk")
            for c in range(4):
                bank, bk = nb()
                mm(bank[:, :], [(pan[:, kc, c * 128:(c + 1) * 128], hT[:, kc, :]) for kc in range(KC)],
                   hTk + [pk], [bk])
                if c < 2:
                    dve(lambda e, c=c, bank=bank: e.scalar_tensor_tensor(
                        out=qdec.ap[:, c, :], in0=bank[:, :], scalar=0.125, in1=eG.ap[:, c, :],
                        op0=ALU.mult, op1=ALU.mult), [bk] + eG.kc(c), qdec.kc(c))
                else:
                    dve(lambda e, c=c, bank=bank: e.tensor_tensor(
                        out=kdec.ap[:, c - 2, :], in0=bank[:, :], in1=enG.ap[:, c - 2, :], op=ALU.mult),
                        [bk] + enG.kc(c - 2), kdec.kc(c - 2))
            for b in range(NB):
                if b % 2 == 0:
                    kb_, kk_ = nb()
                ko = kb_[:, (b % 2) * 256:(b % 2 + 1) * 256]
                mm(ko, [(hT[:, kc, b * 128:(b + 1) * 128], pan[:, kc, 256:512]) for kc in range(KC)],
                   hTk + [pk], [kk_])
                dve(lambda e, b=b, ko=ko: e.tensor_tensor(out=kte.ap[:, b, :], in0=ko, in1=eD.ap[:, b, :],
                                                          op=ALU.mult), [kk_] + eD.kc(b), kte.kc(b))
            panel_load()
            ck(4)
            pan, pk = panel_next("in_v")
            for b in range(NB):
                bank, bk = nb()
                mm(bank[:, :], [(hT[:, kc, b * 128:(b + 1) * 128], pan[:, kc, :]) for kc in range(KC)],
                   hTk + [pk], [bk])
                act(lambda e, b=b, bank=bank: e.activation(out=vtok.ap[:, b, :], in_=bank[:, :], func=AF.Copy),
                    [bk], vtok.kc(b))
            panel_load()
            ck(5)
            pextk = [("pext", c) for c in range(4)]
            for g in range(4):
                src_ap = pext[:, g, :]
                src_k = [("pext", g), "pext_h"]
                L = 16 + T
                sh = 1
                for it in range(g + 1):
                    dst = pa[it % 2]
                    dve(lambda e, src_ap=src_ap, dst=dst, sh=sh, L=L: e.tensor_tensor(
                        out=dst.ap[:, 2 * sh - 1:L], in0=src_ap[:, 2 * sh - 1:L], in1=src_ap[:, sh - 1:L - sh],
                        op=ALU.add),
                        src_k, dst.k())
                    src_ap = dst.ap
                    src_k = dst.k()
                    sh *= 2
                w = 2 ** (g + 1)
                dve(lambda e, g=g, src_ap=src_ap, w=w: e.scalar_tensor_tensor(
                    out=pooled.ap[:, g, :], in0=src_ap[:, 16:16 + T], scalar=1.0 / w, in1=pext[:, g, 16:16 + T],
                    op0=ALU.mult, op1=ALU.subtract), src_k + [("pext", g)], pooled.kc(g))
                if t == 0:
                    dve(lambda e, g=g, src_ap=src_ap: e.tensor_tensor(
                        out=src_ap[:, 16:32], in0=src_ap[:, 16:32], in1=invc[:, g, :], op=ALU.mult),
                        src_k + ["invc"], src_k)
                    dve(lambda e, g=g, src_ap=src_ap: e.tensor_tensor(
                        out=pooled.ap[:, g, 0:16], in0=src_ap[:, 16:32], in1=pext[:, g, 16:32], op=ALU.subtract),
                        src_k + [("pext", g)], pooled.kc(g))
                bank, bk = nb()
                mm(bank[:, :], [(poolw_b[:, g, :], pooled.ap[:, g, :])], pooled.kc(g) + ["poolw_b"], [bk])
                act(lambda e, g=g, bank=bank: e.activation(out=catT.ap[:, g, :], in_=bank[:, :], func=AF.Copy,
                                                           scale=pscale[:, g:g + 1]), [bk, "pscale"], catT.kc(g))
            dve(lambda e: e.tensor_copy(out=pext[:, :, 0:16], in_=pext[:, :, T:T + 16]), pextk + ["pext_h"],
                ["pext_h"])
            ck(6)
            pan, pk = panel_next("in_r")
            for c in range(4):
                bank, bk = nb()
                mm(bank[:, :], [(pan[:, kc, c * 128:(c + 1) * 128], hT[:, kc, :]) for kc in range(KC)],
                   hTk + [pk], [bk])
                act(lambda e, c=c, bank=bank: e.activation(out=sr.ap[:, c, :], in_=bank[:, :], func=AF.Silu),
                    [bk], sr.kc(c))
            panel_load()
            ck(7)
            for hh in range(2):
                for hl in range(2):
                    scb, sck = nb()
                    groups = []
                    for bl in range(2):
                        b = 2 * hh + bl
                        for hf in range(2):
                            c = 2 * b + hf
                            for hp in range(2):
                                groups.append((scb[64 * hf:64 * hf + 64, bl * 128 + hp * 64: bl * 128 + hp * 64 + 64],
                                               [(kdec.ap[64 * hl:64 * hl + 64, hp, c * 64:(c + 1) * 64],
                                                 qdec.ap[64 * hl:64 * hl + 64, hp, c * 64:(c + 1) * 64])]))
                    mm_multi(groups, kdec.k() + qdec.k(), [sck])
                    dve(lambda e, hh=hh, hl=hl, scb=scb: e.tensor_tensor(
                        out=scm.ap[:, 2 * hh:2 * hh + 2, hl * 128:(hl + 1) * 128],
                        in0=scb[:, 0:256].rearrange("p (a b) -> p a b", a=2),
                        in1=cmask2[:, :, :],
                        op=ALU.mult), [sck, "cmask"], scm.kc(2 * hh, 2 * hh + 2))
                ck(8)
                kvb = []
                for hf in range(2):
                    bank, bk = nb()
                    groups = []
                    for bl in range(2):
                        b = 2 * hh + bl
                        for h in range(4):
                            pr, hl = divmod(h, 2)
                            groups.append((bank[64 * hl:64 * hl + 64, bl * 256 + pr * 128: bl * 256 + pr * 128 + 128],
                                           [(kte.ap[64 * hf:64 * hf + 64, b, h * 64:(h + 1) * 64],
                                             vtok.ap[64 * hf:64 * hf + 64, b, h * 128:(h + 1) * 128])]))
                    mm_multi(groups, kte.kc(2 * hh, 2 * hh + 2) + vtok.kc(2 * hh, 2 * hh + 2), [bk])
                    kvb.append((bank, bk))
                ck(9)
                for cl in range(4):
                    c = 4 * hh + cl
                    bl, hf = divmod(cl, 2)
                    bank, bk = kvb[hf]
                    act(lambda e, c=c: e.activation(out=Sbf.ap[:, c, :], in_=Sst[:].rearrange("p a b -> p (a b)"),
                                                    func=AF.Copy), ["Sst"], Sbf.kc(c))
                    for pr in range(2):
                        dve(lambda e, c=c, pr=pr, bank=bank, bl=bl: e.scalar_tensor_tensor(
                            out=Sst[:, pr, :], in0=Sst[:, pr, :], scalar=dec[:, pr, c:c + 1],
                            in1=bank[:, bl * 256 + pr * 128: bl * 256 + pr * 128 + 128],
                            op0=ALU.mult, op1=ALU.add), ["Sst", bk, ("dec", pr)], ["Sst"])
                ck(10)
                oib = []
                for hf in range(2):
                    bank, bk = nb()
                    groups = []
                    for bl in range(2):
                        b = 2 * hh + bl
                        for h in range(4):
                            hp, hl = divmod(h, 2)
                            groups.append((bank[:, bl * 256 + h * 64: bl * 256 + h * 64 + 64],
                                           [(vtok.ap[64 * hf:64 * hf + 64, b, h * 128:(h + 1) * 128],
                                             scm.ap[64 * hf:64 * hf + 64, b, (hl * 2 + hp) * 64:(hl * 2 + hp) * 64 + 64])]))
                    mm_multi(groups, vtok.kc(2 * hh, 2 * hh + 2) + scm.kc(2 * hh, 2 * hh + 2), [bk])
                    oib.append((bank, bk))
                ck(11)
                for hl in range(2):
                    bank, bk = nb()
                    groups = []
                    for cl in range(4):
                        c = 4 * hh + cl
                        for hp in range(2):
                            groups.append((bank[:, cl * 128 + hp * 64: cl * 128 + hp * 64 + 64],
                                           [(Sbf.ap[64 * hl:64 * hl + 64, c, hp * 128:(hp + 1) * 128],
                                             qdec.ap[64 * hl:64 * hl + 64, hp, c * 64:(c + 1) * 64])]))
                    mm_multi(groups, Sbf.kc(4 * hh, 4 * hh + 4) + qdec.k(), [bk])
                    for hp in range(2):
                        act(lambda e, hl=hl, hp=hp, hh=hh, bank=bank: e.activation(
                            out=o_sb.ap[:, 2 * hp + hl, hh * 256:(hh + 1) * 256].rearrange("p (c i) -> p c i", i=64),
                            in_=bank[:, :].rearrange("p (c q i) -> p c q i", q=2, i=64)[:, :, hp, :],
                            func=AF.Copy), [bk], o_sb.kc(2 * hp + hl))
                ck(12)
                for hf in range(2):
                    bank, bk = oib[hf]
                    for bl in range(2):
                        cl = 2 * bl + hf
                        dve(lambda e, hh=hh, bl=bl, cl=cl, bank=bank: e.tensor_tensor(
                            out=o_sb.ap[:, :, hh * 256 + cl * 64: hh * 256 + cl * 64 + 64],
                            in0=o_sb.ap[:, :, hh * 256 + cl * 64: hh * 256 + cl * 64 + 64],
                            in1=bank[:, bl * 256:(bl + 1) * 256].rearrange("p (h i) -> p h i", i=64),
                            op=ALU.add), [bk] + o_sb.k(), o_sb.k())
            for h in range(4):
                act(lambda e, h=h: e.activation(out=sq.ap[:, h, :], in_=o_sb.ap[:, h, :], func=AF.Square),
                    o_sb.kc(h), sq.kc(h))
            ck(13)
            for h in range(4):
                bank, bk = nb()
                mm(bank[:, :], [(ones_f[:, :], sq.ap[:, h, :])], sq.kc(h) + ["ones_f"], [bk])
                r_ = rb[h % 2]
                act(lambda e, bank=bank, r_=r_: e.activation(out=r_.ap, in_=bank[:, :], func=AF.Ln,
                                                             bias=epsc[:, 0:1]), [bk, "epsc"], r_.k())
                act(lambda e, r_=r_: e.activation(out=r_.ap, in_=r_.ap, func=AF.Exp, scale=-0.5), r_.k(), r_.k())
                dve(lambda e, h=h, r_=r_: e.scalar_tensor_tensor(
                    out=gl.ap, in0=o_sb.ap[:, h, :], scalar=gnw[:, 0:1], in1=r_.ap, op0=ALU.mult, op1=ALU.mult),
                    o_sb.kc(h) + r_.k() + ["gnw"], gl.k())
                dve(lambda e, h=h: e.tensor_tensor(out=catT.ap[:, 4 + h, :], in0=gl.ap, in1=sr.ap[:, h, :],
                                                   op=ALU.mult), gl.k() + sr.kc(h), catT.kc(4 + h))
            ck(14)
            proj_residual(catT, ["wout0", "wout1"], xb, xk)

            S.mute = en is not None and "xat" not in en
            norm_T(lambda b: xb[:, b, :], lambda b: [(xk, b)], NB, 1)
            for i in range(2):
                pan, pk = panel_next("wq%d" % i)
                for c in range(4):
                    bank, bk = nb()
                    mm(bank[:, :], [(pan[:, kc, c * 128:(c + 1) * 128], hT[:, kc, :]) for kc in range(KC)],
                       hTk + [pk], [bk])
                    cc = i * 4 + c
                    if c % 2 == 0:
                        act(lambda e, cc=cc, bank=bank: e.activation(out=qxT.ap[:, cc, :], in_=bank[:, :],
                                                                     func=AF.Copy), [bk], qxT.kc(cc))
                    else:
                        dve(lambda e, cc=cc, bank=bank: e.tensor_copy(out=qxT.ap[:, cc, :], in_=bank[:, :]),
                            [bk], qxT.kc(cc))
                panel_load()
            for h in range(4):
                for mc in range(2):
                    bank, bk = nb()
                    mm(bank[:, :], [(KT[:, 2 * h + dc, mc * 128:(mc + 1) * 128], qxT.ap[:, 2 * h + dc, :])
                                    for dc in range(2)],
                       qxT.kc(2 * h, 2 * h + 2) + [("KT", 2 * h), ("KT", 2 * h + 1)], [bk])
                    act(lambda e, h=h, mc=mc, bank=bank: e.activation(out=PT.ap[:, 2 * h + mc, :], in_=bank[:, :],
                                                                      func=AF.Exp, scale=1.0 / 16.0),
                        [bk], PT.kc(2 * h + mc))
                smb, smk = nb()
                mm(smb[:, :], [(ones_b[:, :], PT.ap[:, 2 * h + mc, :]) for mc in range(2)],
                   PT.kc(2 * h, 2 * h + 2) + ["ones_b"], [smk])
                r_ = rs[h % 2]
                dve(lambda e, smb=smb, r_=r_: e.reciprocal(out=r_.ap, in_=smb[:, :]), [smk], r_.k())
                for dc in range(2):
                    bank, bk = nb()
                    mm(bank[:, :], [(Vm[:, mc, h * 256 + dc * 128: h * 256 + dc * 128 + 128], PT.ap[:, 2 * h + mc, :])
                                    for mc in range(2)],
                       PT.kc(2 * h, 2 * h + 2) + [("Vm", 0), ("Vm", 1)], [bk])
                    dve(lambda e, h=h, dc=dc, bank=bank, r_=r_: e.tensor_tensor(
                        out=oxT.ap[:, 2 * h + dc, :], in0=bank[:, :], in1=r_.ap, op=ALU.mult),
                        [bk] + r_.k(), oxT.kc(2 * h + dc))
            proj_residual(oxT, ["wo0", "wo1"], xb, xk)

            S.mute = en is not None and "ffn" not in en
            norm_T(lambda b: xb[:, b, :], lambda b: [(xk, b)], NB, 3)
            for j in range(11):
                pan, pk = panel_next("up%d" % j)
                ys = []
                for r4 in range(4):
                    q = 4 * j + r4
                    bank, bk = nb()
                    mm(bank[:, :], [(pan[:, kc, r4 * 128:(r4 + 1) * 128], hT[:, kc, :]) for kc in range(KC)],
                       hTk + [pk], [bk])
                    ub = ubuf[q % 4]
                    yb = ybuf[q % 4]
                    act(lambda e, q=q, ub=ub: e.activation(out=ub.ap[:, 0:2], in_=halo[:, q, :], func=AF.Copy),
                        [("halo", q), "halo"], ub.k())
                    act(lambda e, ub=ub, bank=bank: e.activation(out=ub.ap[:, 2:2 + T], in_=bank[:, :], func=AF.Copy),
                        [bk], ub.k())
                    act(lambda e, q=q, yb=yb, bank=bank: e.activation(
                        out=yb.ap, in_=bank[:, :], func=AF.Identity, scale=cw[:, q, 2:3], bias=cb[:, q:q + 1]),
                        [bk, "cw", "cb"], yb.k())
                    act(lambda e, q=q, bank=bank: e.activation(out=halo[:, q, :], in_=bank[:, T - 2:T], func=AF.Copy),
                        [bk], [("halo", q)])
                    dve(lambda e, q=q, ub=ub, yb=yb: e.scalar_tensor_tensor(
                        out=yb.ap, in0=ub.ap[:, 1:1 + T], scalar=cw[:, q, 1:2], in1=yb.ap,
                        op0=ALU.mult, op1=ALU.add), ub.k() + yb.k() + ["cw"], yb.k())
                    dve(lambda e, q=q, ub=ub, yb=yb: e.scalar_tensor_tensor(
                        out=yb.ap, in0=ub.ap[:, 0:T], scalar=cw[:, q, 0:1], in1=yb.ap,
                        op0=ALU.mult, op1=ALU.add), ub.k() + yb.k() + ["cw"], yb.k())
                    ys.append(yb)
                panel_load()
                for r2 in range(2):
                    ch = 2 * j + r2
                    sgb = sg[ch % 2]
                    yg, yv = ys[r2], ys[2 + r2]
                    act(lambda e, yg=yg, sgb=sgb: e.activation(out=sgb.ap, in_=yg.ap, func=AF.Silu),
                        yg.k(), sgb.k())
                    dve(lambda e, ch=ch, sgb=sgb, yv=yv: e.tensor_tensor(out=actT.ap[:, ch, :], in0=sgb.ap,
                                                                         in1=yv.ap, op=ALU.mult),
                        sgb.k() + yv.k(), actT.kc(ch))
            for nh in range(2):
                banks = [nb() for _ in range(NB)]
                for kg in range(3):
                    pan, pk = panel_next("dn%d_%d" % (nh, kg))
                    nk = 8 if kg < 2 else 6
                    for b in range(NB):
                        bank, bk = banks[b]

                        def fn(e, b=b, bank=bank, kg=kg, nk=nk, pan=pan):
                            ins = None
                            for kc in range(nk):
                                ins = e.matmul(bank[:, :], actT.ap[:, kg * 8 + kc, b * 128:(b + 1) * 128],
                                               pan[:, kc, :], start=(kg == 0 and kc == 0),
                                               stop=(kg == 2 and kc == nk - 1))
                            return ins
                        S.op("pe", fn, actT.kc(kg * 8, kg * 8 + nk) + [pk], [bk])
                    panel_load()
                for b in range(NB):
                    bank, bk = banks[b]
                    dve(lambda e, b=b, nh=nh, bank=bank, xb=xb: e.tensor_tensor(
                        out=xb[:, b, nh * 512:(nh + 1) * 512], in0=xb[:, b, nh * 512:(nh + 1) * 512],
                        in1=bank[:, :], op=ALU.add), [bk, (xk, b)], [(xk, b)])

            S.mute = en is not None and "fin" not in en
            for b in range(NB):
                act(lambda e, b=b, xb=xb: e.activation(out=junk[:], in_=xb[:, b, :], func=AF.Square,
                                                accum_out=ss[:, b:b + 1]), [(xk, b)], ["junk", ("ss", b)])
            act(lambda e: e.activation(out=rstd[:, :], in_=ss[:, :], func=AF.Ln, scale=1.0 / D, bias=epsc[:, 0:1]),
                [("ss", b) for b in range(NB)] + ["epsc"], ["rstd"])
            act(lambda e: e.activation(out=rstd[:, :], in_=rstd[:, :], func=AF.Exp, scale=-0.5), ["rstd"], ["rstd"])
            for b in range(NB):
                dve(lambda e, b=b, xb=xb: e.scalar_tensor_tensor(out=xb[:, b, :], in0=xb[:, b, :], scalar=rstd[:, b:b + 1],
                                                          in1=wf_bc[:, :], op0=ALU.mult, op1=ALU.mult),
                    [(xk, b), "rstd", "wf_bc"], [(xk, b)])
            S.mute = False
            S.dma("sp", xk, lambda e, xb=xb, row0=row0: e.dma_start(
                out=out_d.ap()[row0:row0 + T, :].rearrange("(b p) d -> p b d", p=128), in_=xb[:]),
                reads=xkeys, writes=[("out", tile_idx)])
            tile_idx += 1

    S.wait_all("sp", [S.lastw[("out", i)] for i in range(tile_idx)])
    S.emit()
    return nc


def _prep_shared(inp):
    f = lambda a: np.ascontiguousarray(np.asarray(a, dtype=np.float32))
    w_in = f(inp["w_in"])[0]
    tri, tris, cmask, ident, invc = _consts()
    col = lambda v, n: np.ascontiguousarray(f(v).reshape(n, 128).T)
    nw = np.stack([col(inp["norm_mix_w"][0], 8), col(inp["norm_xattn_w"][0], 8),
                   col(inp["norm_mem_w"][0], 8), col(inp["norm_ffn_w"][0], 8)], axis=1)
    wg = w_in[:, 1536:1552].reshape(8, 128, 16).transpose(1, 0, 2)
    poolw = f(inp["pool_w"])[0].transpose(1, 0, 2)
    gkw = np.concatenate([f(inp["gk_w2"])[0], f(inp["gk_b"])[0][None, :]], axis=0)
    qcols = np.array([_chunk_col(q) for q in range(NCH)])
    idx = qcols[:, None] + np.arange(128)[None, :]
    cwf = f(inp["ffn_conv_w"])[0]
    cw = cwf[:, idx].transpose(2, 1, 0)
    cb = f(inp["ffn_conv_b"])[0][idx].T
    shared = {
        "wpan": _pack_weights(w_in, f(inp["w_out"])[0], f(inp["xattn_wq"])[0], f(inp["xattn_wkv"])[0],
                              f(inp["xattn_wo"])[0], f(inp["ffn_w_up"])[0], f(inp["ffn_w_down"])[0]),
        "wg": np.ascontiguousarray(wg).reshape(128, 128),
        "nw": np.ascontiguousarray(nw).reshape(128, 32),
        "wf": f(inp["norm_final_w"]).reshape(1, D),
        "poolw": np.ascontiguousarray(poolw).reshape(128, 512),
        "pscale": col(inp["pool_scale"][0], 4),
        "gkw": np.ascontiguousarray(gkw),
        "gnw": f(inp["gla_norm_w"])[0].reshape(128, 1),
        "cw": np.ascontiguousarray(cw).reshape(128, NCH * 3),
        "cb": np.ascontiguousarray(cb),
        "tri": tri, "tris": tris, "cmask": cmask, "ident": ident, "invc": invc,
    }
    return shared


def kernel(**inputs):
    x = np.asarray(inputs["x"], dtype=np.float32)
    mem = np.asarray(inputs["mem"], dtype=np.float32)
    shared = _prep_shared(inputs)
    nc = build_program()
    in_maps = []
    for c in range(NCORES):
        m = dict(shared)
        m["x"] = np.ascontiguousarray(x[c * SEQ_PER_CORE:(c + 1) * SEQ_PER_CORE]).reshape(SEQ_PER_CORE * SEQ, D)
        m["mem"] = np.ascontiguousarray(mem[c * SEQ_PER_CORE:(c + 1) * SEQ_PER_CORE]).reshape(SEQ_PER_CORE * NMEM, D)
        in_maps.append(m)
    res = run_bass_kernel_spmd(nc, in_maps, core_ids=list(range(NCORES)))
    out = np.concatenate([r["out"].reshape(SEQ_PER_CORE, SEQ, D) for r in res.results], axis=0)
    return out.astype(np.float32)
```

```python
import contextlib
import numpy as np
import concourse.bass as bass
import concourse.mybir as mybir
from concourse.bass_utils import run_bass_kernel_spmd

F32 = mybir.dt.float32
BF16 = mybir.dt.bfloat16
AF = mybir.ActivationFunctionType
ALU = mybir.AluOpType

D = 1024
SEQ = 2048
NMEM = 256
T = 512
NB = 4
KC = 8
DFF = 2816
NCH = 44
EPS = 1e-6
NCORES = 8
SEQ_PER_CORE = 4
RING = 4
NCV = 8
STRICT = True


class Sched:
    ENGS = ("pe", "act", "dve", "pool", "sp")

    def __init__(self, nc):
        self.nc = nc
        self.ops = {e: [] for e in self.ENGS}
        self.cnt = {}
        self.seen = {e: {} for e in self.ENGS}
        self.lastw = {}
        self.readers = {}
        self.chan_last = {}
        self.mute = False

    def _deps(self, reads, writes, eng=None):
        deps = []
        own = ("c", eng)
        for b in reads:
            t = self.lastw.get(b)
            if t is not None:
                deps.append(t)
            if isinstance(b, tuple) and b[0] in ("ps", "pt"):
                r = self.readers.get(b)
                if r:
                    deps.extend((k, v) for k, v in r.items() if k != own)
        for b in writes:
            t = self.lastw.get(b)
            if t is not None and (STRICT or t[0] != own):
                deps.append(t)
            r = self.readers.get(b)
            if r:
                deps.extend((k, v) for k, v in r.items() if (STRICT or k != own))
        return deps

    def _commit(self, tok, reads, writes):
        k, v = tok
        for b in reads:
            d = self.readers.setdefault(b, {})
            if d.get(k, 0) < v:
                d[k] = v
        for b in writes:
            self.lastw[b] = tok
            self.readers[b] = {}

    def _waits(self, eng, deps):
        need = {}
        for (k, v) in deps:
            if eng == "pe" and k == ("c", "pe"):
                continue
            if need.get(k, 0) < v:
                need[k] = v
        out = []
        seen = self.seen[eng]
        for k, v in need.items():
            if seen.get(k, 0) < v:
                seen[k] = v
                out.append((k, v))
        return out

    def op(self, eng, fn, reads=(), writes=()):
        if self.mute:
            return None
        waits = self._waits(eng, self._deps(reads, writes, eng))
        k = ("c", eng)
        self.cnt[k] = self.cnt.get(k, 0) + 1
        tok = (k, self.cnt[k])
        self._commit(tok, reads, writes)
        self.ops[eng].append((waits, fn, k, 1))
        return tok

    def dma(self, eng, chan, fn, reads=(), writes=()):
        if self.mute:
            return None
        deps = self._deps(reads, writes)
        k = ("d", chan)
        if k in self.chan_last:
            deps.append(self.chan_last[k])
        waits = self._waits(eng, deps)
        self.cnt[k] = self.cnt.get(k, 0) + 16
        tok = (k, self.cnt[k])
        self.chan_last[k] = tok
        self._commit(tok, reads, writes)
        self.ops[eng].append((waits, fn, k, 16))
        return tok

    def wait_all(self, eng, toks):
        waits = self._waits(eng, [t for t in toks if t is not None])
        self.ops[eng].append((waits, None, None, 0))

    def emit(self):
        nc = self.nc
        sems = {}
        with contextlib.ExitStack() as st:
            for k in self.cnt:
                sems[k] = st.enter_context(nc.semaphore("s_%s_%s" % k))
            block = st.enter_context(nc.Block())

            def run(engobj, e):
                for (waits, fn, semk, incv) in self.ops[e]:
                    for (k, v) in waits:
                        engobj.wait_ge(sems[k], v)
                    if fn is not None:
                        ins = fn(engobj)
                        ins.then_inc(sems[semk], incv)

            @block.tensor
            def _(eng):
                run(eng, "pe")

            @block.scalar
            def _(eng):
                run(eng, "act")

            @block.vector
            def _(eng):
                run(eng, "dve")

            @block.gpsimd
            def _(eng):
                run(eng, "pool")

            @block.sync
            def _(eng):
                run(eng, "sp")


def _chunk_col(q):
    j, r = divmod(q, 4)
    return (2 * j + (r % 2)) * 128 + (DFF if r >= 2 else 0)


def _panel(W, r0, nk, cols):
    P = np.zeros((128, KC, 512), np.float32)
    blk = W[r0:r0 + nk * 128][:, cols]
    P[:, :nk, :] = blk.reshape(nk, 128, 512).transpose(1, 0, 2)
    return P.reshape(128, KC * 512)


PANEL_NAMES = (["kv%d" % i for i in range(4)] + ["in_p", "in_qk", "in_v", "in_r", "wout0", "wout1",
               "wq0", "wq1", "wo0", "wo1"] + ["up%d" % i for i in range(11)] +
               ["dn%d_%d" % (nh, kg) for nh in range(2) for kg in range(3)])
PID = {n: i for i, n in enumerate(PANEL_NAMES)}
NPAN = len(PANEL_NAMES)
TILE_PANELS = (["in_p", "in_qk", "in_v", "in_r", "wout0", "wout1", "wq0", "wq1", "wo0", "wo1"] +
               ["up%d" % i for i in range(11)] + ["dn%d_%d" % (nh, kg) for nh in range(2) for kg in range(3)])


def _pack_weights(w_in, w_out, wq, wkv, wo, w_up, w_down):
    ar = np.arange(512)
    out = np.zeros((NPAN, 128, KC * 512), np.float32)
    for i in range(4):
        out[PID["kv%d" % i]] = _panel(wkv, 0, 8, ar + 512 * i)
    out[PID["in_p"]] = _panel(w_in, 0, 8, ar)
    out[PID["in_qk"]] = _panel(w_in, 0, 8, ar + 512)
    out[PID["in_v"]] = _panel(w_in, 0, 8, ar + 1024)
    out[PID["in_r"]] = _panel(w_in, 0, 8, ar + 1552)
    for i in range(2):
        out[PID["wout%d" % i]] = _panel(w_out, 0, 8, ar + 512 * i)
        out[PID["wq%d" % i]] = _panel(wq, 0, 8, ar + 512 * i)
        out[PID["wo%d" % i]] = _panel(wo, 0, 8, ar + 512 * i)
    for j in range(11):
        cols = np.concatenate([np.arange(128) + _chunk_col(4 * j + r) for r in range(4)])
        out[PID["up%d" % j]] = _panel(w_up, 0, 8, cols)
    for nh in range(2):
        for kg in range(3):
            nk = 8 if kg < 2 else 6
            out[PID["dn%d_%d" % (nh, kg)]] = _panel(w_down, kg * 1024, nk, ar + 512 * nh)
    return out


def _consts():
    s = np.arange(128)[:, None]
    t = np.arange(128)[None, :]
    same = (s // 64) == (t // 64)
    tri = np.where(same & (s <= t), -1.0 / 16.0, 0.0).astype(np.float32)
    tris = np.where(same & (s > t), -1.0 / 16.0, 0.0).astype(np.float32)
    j = (np.arange(128) % 64)[:, None, None]
    i = np.arange(64)[None, None, :]
    cmask = np.broadcast_to((j <= i), (128, 4, 64)).astype(np.float32).reshape(128, 256)
    ident = np.eye(128, dtype=np.float32)
    invc = np.zeros((128, 4, 16), np.float32)
    tt = np.arange(16)
    for g, w in enumerate((2, 4, 8, 16)):
        invc[:, g, :] = 1.0 / np.minimum(tt + 1, w)
    return tri, tris, cmask, ident, invc.reshape(128, 64)


def build_program(nseq=SEQ_PER_CORE, ntile=SEQ // T, dbg=None):
    nc = bass.Bass("TRN2", target_bir_lowering=False)
    S = Sched(nc)
    en = dbg
    import os as _os
    MIXSTOP = int(_os.environ.get("MIXSTOP", "99"))

    def ck(n):
        if n > MIXSTOP:
            S.mute = True

    def din(name, shape):
        return nc.dram_tensor(name, list(shape), F32, kind="ExternalInput")

    x_d = din("x", [nseq * SEQ, D])
    mem_d = din("mem", [nseq * NMEM, D])
    wpan_d = din("wpan", [NPAN, 128, KC * 512])
    wg_d = din("wg", [128, KC * 16])
    nw_d = din("nw", [128, 4 * KC])
    wf_d = din("wf", [1, D])
    poolw_d = din("poolw", [128, 4 * 128])
    pscale_d = din("pscale", [128, 4])
    gkw_d = din("gkw", [17, 256])
    gnw_d = din("gnw", [128, 1])
    cw_d = din("cw", [128, NCH * 3])
    cb_d = din("cb", [128, NCH])
    tri_d = din("tri", [128, 128])
    tris_d = din("tris", [128, 128])
    cmask_d = din("cmask", [128, 256])
    ident_d = din("ident", [128, 128])
    invc_d = din("invc", [128, 64])
    out_d = nc.dram_tensor("out", [nseq * SEQ, D], F32, kind="ExternalOutput")
    wbf_d = nc.dram_tensor("wbf", [NPAN, 128, KC * 512], BF16, kind="Internal")

    def sb(name, shape, dt=F32):
        return nc.alloc_sbuf_tensor("s_" + name, list(shape), dt)

    xbuf = [sb("xb%d" % i, [128, NB, D]) for i in range(2)]
    ring = [sb("ring%d" % i, [128, KC, 512], BF16) for i in range(RING)]
    hT = sb("hT", [128, KC, T], BF16)
    hb = sb("hb", [128, NB, D], BF16)
    junk = sb("junk", [128, D], BF16)
    ss = sb("ss", [128, 4])
    rstd = sb("rstd", [128, 4])
    epsc = sb("epsc", [128, 1])
    wf_bc = sb("wf_bc", [128, D])
    nw = sb("nw", [128, 4, KC])
    wg_f = sb("wg_f", [128, KC, 16])
    wg_b = sb("wg_b", [128, KC, 16], BF16)
    poolw_f = sb("poolw_f", [128, 4, 128])
    poolw_b = sb("poolw_b", [128, 4, 128], BF16)
    pscale = sb("pscale", [128, 4])
    gkw = sb("gkw", [17, 256])
    gnw = sb("gnw", [128, 1])
    cw = sb("cw", [128, NCH, 3])
    cb = sb("cb", [128, NCH])
    tri = sb("tri", [128, 128])
    tris = sb("tris", [128, 128])
    cmask = sb("cmask", [128, 4, 64])
    cmask2 = cmask[:].rearrange("p (a b) c -> p a (b c)", a=2)
    ident_f = sb("ident_f", [128, 128])
    ident_b = sb("ident_b", [128, 128], BF16)
    invc = sb("invc", [128, 4, 16])
    ones_f = sb("ones_f", [128, 128])
    ones_b = sb("ones_b", [128, 128], BF16)
    KT = sb("KT", [128, 8, NMEM], BF16)
    Vm = sb("Vm", [128, 2, D], BF16)
    pext = sb("pext", [128, 4, 16 + T])
    Sst = sb("Sst", [128, 2, 128])
    halo = sb("halo", [128, NCH, 2])
    dec = sb("dec", [128, 2, 8])
    gT = sb("gT", [17, T])
    m_sb = sb("m_sb", [128, 2, D])

    ARENA_KB = 80
    arena = sb("arena", [128, ARENA_KB * 256])

    class AV:
        def __init__(self, off_kb, shape, dt):
            esz = 4 if dt == F32 else 2
            n = int(np.prod(shape))
            self.off = off_kb * 1024
            self.esz = esz
            self.n = n
            assert self.off + n * esz <= ARENA_KB * 1024, (off_kb, shape)
            a = arena[:, self.off // 4:(self.off + n * esz + 3) // 4]
            if dt != F32:
                a = a.bitcast(dt)
            if len(shape) == 2:
                a = a.rearrange("p (a b) -> p a b", a=shape[0])
            elif len(shape) == 3:
                a = a.rearrange("p (a b c) -> p a b c", a=shape[0], b=shape[1])
            self.ap = a
            self.shape = shape

        def k(self, lo=0, hi=None):
            hi = self.n if hi is None else hi
            b0 = (self.off + lo * self.esz) // 1024
            b1 = (self.off + hi * self.esz - 1) // 1024
            return [("ar", i) for i in range(b0, b1 + 1)]

        def kc(self, i, j=None):
            per = self.n // self.shape[0]
            j = i + 1 if j is None else j
            return self.k(i * per, j * per)

    catT = AV(0, [8, T], BF16)
    o_sb = AV(8, [4, T], F32)
    sq = AV(16, [4, T], F32)
    rb = [AV(24 + 2 * i, [T], F32) for i in range(2)]
    gl = AV(28, [T], F32)
    lg = AV(30, [4, 256], F32)
    eD = AV(34, [4, 256], F32)
    eG = AV(38, [2, T], F32)
    enG = AV(42, [2, T], F32)
    qdec = AV(46, [2, T], BF16)
    kdec = AV(48, [2, T], BF16)
    kte = AV(50, [4, 256], BF16)
    vtok = AV(52, [4, T], BF16)
    scm = AV(56, [4, 256], BF16)
    sr = AV(58, [4, T], F32)
    pooled = AV(66, [4, T], BF16)
    pa = [AV(70 + 3 * i, [16 + T], F32) for i in range(2)]
    Sbf = AV(76, [8, 256], BF16)
    oxT = AV(0, [8, T], BF16)
    qxT = AV(8, [8, T], BF16)
    PT = AV(16, [8, T], BF16)
    rs = [AV(24 + 2 * i, [T], F32) for i in range(2)]
    actT = AV(8, [22, T], BF16)
    ubuf = [AV(30 + 3 * i, [2 + T], F32) for i in range(4)]
    ybuf = [AV(42 + 2 * i, [T], F32) for i in range(4)]
    sg = [AV(50 + 2 * i, [T], F32) for i in range(2)]

    NPS = 6
    psb = [nc.alloc_psum_tensor("ps%d" % i, [128, 512], F32) for i in range(NPS)]
    ptb = [nc.alloc_psum_tensor("pt%d" % i, [128, 1024], BF16) for i in range(2)]
    rr = {"ps": 0, "pt": 0}

    def nb():
        i = rr["ps"]
        rr["ps"] = (i + 1) % NPS
        return psb[i], ("ps", i)

    def npt():
        i = rr["pt"]
        rr["pt"] = (i + 1) % 2
        return ptb[i], ("pt", i)

    def act(fn, reads, writes):
        return S.op("act", fn, reads, writes)

    def dve(fn, reads, writes):
        return S.op("dve", fn, reads, writes)

    def pool(fn, reads, writes):
        return S.op("pool", fn, reads, writes)

    def mm(out_ap, pairs, reads, writes):
        n = len(pairs)

        def fn(e):
            ins = None
            for i, (l, r) in enumerate(pairs):
                ins = e.matmul(out_ap, l, r, start=(i == 0), stop=(i == n - 1))
            return ins
        return S.op("pe", fn, reads, writes)

    def mm_multi(groups, reads, writes):
        def fn(e):
            ins = None
            for out_ap, pairs in groups:
                n = len(pairs)
                for i, (l, r) in enumerate(pairs):
                    ins = e.matmul(out_ap, l, r, start=(i == 0), stop=(i == n - 1))
            return ins
        return S.op("pe", fn, reads, writes)

    cch = [0]

    def cload(dst_ap, src_ap, key):
        ch = "c%d" % (cch[0] % 4)
        cch[0] += 1
        S.dma("sp", ch, lambda e: e.dma_start(out=dst_ap, in_=src_ap), writes=[key])

    cload(nw[:], nw_d.ap().rearrange("p (a b) -> p a b", a=4), "nw")
    cload(wg_f[:], wg_d.ap().rearrange("p (a b) -> p a b", a=KC), "wg_f")
    cload(wf_bc[:], wf_d.ap().partition_broadcast(128), "wf_bc")
    cload(poolw_f[:], poolw_d.ap().rearrange("p (a b) -> p a b", a=4), "poolw_f")
    cload(pscale[:], pscale_d.ap(), "pscale")
    cload(gkw[:], gkw_d.ap(), "gkw")
    cload(gnw[:], gnw_d.ap(), "gnw")
    cload(cw[:], cw_d.ap().rearrange("p (a b) -> p a b", a=NCH), "cw")
    cload(cb[:], cb_d.ap(), "cb")
    cload(tri[:], tri_d.ap(), "tri")
    cload(tris[:], tris_d.ap(), "tris")
    cload(cmask[:], cmask_d.ap().rearrange("p (a b) -> p a b", a=4), "cmask")
    cload(ident_f[:], ident_d.ap(), "ident_f")
    cload(invc[:], invc_d.ap().rearrange("p (a b) -> p a b", a=4), "invc")
    dve(lambda e: e.tensor_copy(out=ident_b[:], in_=ident_f[:]), ["ident_f"], ["ident_b"])
    dve(lambda e: e.tensor_copy(out=wg_b[:], in_=wg_f[:]), ["wg_f"], ["wg_b"])
    dve(lambda e: e.tensor_copy(out=poolw_b[:], in_=poolw_f[:]), ["poolw_f"], ["poolw_b"])
    dve(lambda e: e.memset(ones_f[:], 1.0 / 128.0), [], ["ones_f"])
    dve(lambda e: e.memset(ones_b[:], 1.0), [], ["ones_b"])
    dve(lambda e: e.memset(epsc[:], EPS), [], ["epsc"])
    dve(lambda e: e.memset(gT[:], 1.0), [], ["gT"])

    for j in range(NPAN):
        S.dma("pool", "cv%d" % (j % NCV),
              lambda e, j=j: e.dma_start(out=wbf_d.ap()[j], in_=wpan_d.ap()[j]),
              writes=[("wbf", j)])

    seq_panels = []
    for s in range(nseq):
        seq_panels += ["kv%d" % i for i in range(4)]
        for t in range(ntile):
            seq_panels += TILE_PANELS
    pstate = {"next_load": 0, "next_use": 0}

    def panel_load():
        i = pstate["next_load"]
        if i >= len(seq_panels):
            return
        pstate["next_load"] = i + 1
        slot = i % RING
        pid = PID[seq_panels[i]]
        m_ = S.mute
        S.mute = False
        S.dma("sp", "ring%d" % slot,
              lambda e: e.dma_start(out=ring[slot][:].rearrange("p a b -> p (a b)"), in_=wbf_d.ap()[pid]),
              reads=[("wbf", pid)], writes=[("ring", slot)])
        S.mute = m_

    def panel_next(expect):
        i = pstate["next_use"]
        assert seq_panels[i] == expect, (seq_panels[i], expect)
        pstate["next_use"] = i + 1
        slot = i % RING
        return ring[slot], ("ring", slot)

    for _ in range(RING):
        panel_load()

    def norm_T(xap_of_block, xkeys_of_block, nblk, widx):
        ntok = nblk * 128
        for b in range(nblk):
            xin = xap_of_block(b)
            act(lambda e, b=b, xin=xin: e.activation(out=junk[:], in_=xin, func=AF.Square,
                                                     accum_out=ss[:, b:b + 1]),
                xkeys_of_block(b), ["junk", ("ss", b)])
        sskeys = [("ss", b) for b in range(nblk)]
        act(lambda e: e.activation(out=rstd[:, 0:nblk], in_=ss[:, 0:nblk], func=AF.Ln,
                                   scale=1.0 / D, bias=epsc[:, 0:1]), sskeys + ["epsc"], [("rstd", b_) for b_ in range(4)])
        act(lambda e: e.activation(out=rstd[:, 0:nblk], in_=rstd[:, 0:nblk], func=AF.Exp, scale=-0.5),
            [("rstd", b_) for b_ in range(4)], [("rstd", b_) for b_ in range(4)])
        for b in range(nblk):
            xin = xap_of_block(b)
            dve(lambda e, b=b, xin=xin: e.tensor_scalar(out=hb[:, b, :], in0=xin,
                                                        scalar1=rstd[:, b:b + 1], scalar2=None, op0=ALU.mult),
                xkeys_of_block(b) + [("rstd", b)], [("hb", b)])
        for kc in range(KC):
            pt, ptk = npt()

            def fn(e, kc=kc, pt=pt):
                ins = None
                for b in range(nblk):
                    ins = e.transpose(out=pt[:, b * 128:(b + 1) * 128],
                                      in_=hb[:, b, kc * 128:(kc + 1) * 128], identity=ident_b[:])
                return ins
            S.op("pe", fn, [("hb", b) for b in range(nblk)] + ["ident_b"], [ptk])
            if kc % 2 == 0:
                act(lambda e, kc=kc, pt=pt: e.activation(out=hT[:, kc, 0:ntok], in_=pt[:, 0:ntok], func=AF.Copy,
                                                         scale=nw[:, widx, kc:kc + 1]),
                    [ptk, "nw"], [("hT", kc)])
            else:
                dve(lambda e, kc=kc, pt=pt: e.tensor_scalar(out=hT[:, kc, 0:ntok], in0=pt[:, 0:ntok],
                                                            scalar1=nw[:, widx, kc:kc + 1], scalar2=None,
                                                            op0=ALU.mult),
                    [ptk, "nw"], [("hT", kc)])

    def norm_x(xb_, xk_, widx):
        for b in range(NB):
            xin = xb_[:, b, :]
            act(lambda e, b=b, xin=xin: e.activation(out=junk[:], in_=xin, func=AF.Square,
                                                     accum_out=ss[:, b:b + 1]), [(xk_, b)], ["junk", ("ss", b)])
            act(lambda e, b=b: e.activation(out=rstd[:, b:b + 1], in_=ss[:, b:b + 1], func=AF.Ln,
                                            scale=1.0 / D, bias=epsc[:, 0:1]), [("ss", b), "epsc"], [("rstd", b)])
            act(lambda e, b=b: e.activation(out=rstd[:, b:b + 1], in_=rstd[:, b:b + 1], func=AF.Exp, scale=-0.5),
                [("rstd", b)], [("rstd", b)])
            dve(lambda e, b=b, xin=xin: e.tensor_scalar(out=hb[:, b, :], in0=xin, scalar1=rstd[:, b:b + 1],
                                                        scalar2=None, op0=ALU.mult),
                [(xk_, b), ("rstd", b)], [("hb", b)])
            pt, ptk = npt()

            def fn(e, b=b, pt=pt):
                ins = None
                for kc in range(KC):
                    ins = e.transpose(out=pt[:, kc * 128:(kc + 1) * 128],
                                      in_=hb[:, b, kc * 128:(kc + 1) * 128], identity=ident_b[:])
                return ins
            S.op("pe", fn, [("hb", b), "ident_b"], [ptk])
            dve(lambda e, b=b, pt=pt: e.tensor_tensor(
                out=hT[:, :, b * 128:(b + 1) * 128],
                in0=pt[:, :].rearrange("p (k t) -> p k t", k=KC),
                in1=nw[:, widx, :].unsqueeze(2).broadcast_to([128, KC, 128]), op=ALU.mult),
                [ptk, "nw"], [("hT", kc) for kc in range(KC)])

    hTk = [("hT", kc) for kc in range(KC)]

    def proj_residual(src, names, xb, xk):
        for nh, nm in enumerate(names):
            pan, pk = panel_next(nm)
            for b in range(NB):
                bank, bk = nb()
                mm(bank[:, :], [(src.ap[:, kc, b * 128:(b + 1) * 128], pan[:, kc, :]) for kc in range(KC)],
                   src.k() + [pk], [bk])
                dve(lambda e, b=b, nh=nh, bank=bank: e.tensor_tensor(
                    out=xb[:, b, nh * 512:(nh + 1) * 512], in0=xb[:, b, nh * 512:(nh + 1) * 512],
                    in1=bank[:, :], op=ALU.add), [bk, (xk, b)], [(xk, b)])
            panel_load()

    def load_mem(s_):
        S.dma("sp", "mem", lambda e: e.dma_start(
            out=m_sb[:], in_=mem_d.ap()[s_ * NMEM:(s_ + 1) * NMEM, :].rearrange("(b p) d -> p b d", p=128)),
            writes=["m_sb"])

    def load_x(s_, t_):
        g = s_ * ntile + t_
        r0 = s_ * SEQ + t_ * T
        S.dma("sp", "x%d" % (g % 2), lambda e: e.dma_start(
            out=xbuf[g % 2][:], in_=x_d.ap()[r0:r0 + T, :].rearrange("(b p) d -> p b d", p=128)),
            writes=[("x%d" % (g % 2), b) for b in range(NB)])

    tile_idx = 0
    for s in range(nseq):
        if s == 0:
            load_mem(0)
            load_x(0, 0)
        S.mute = en is not None and "kv" not in en
        norm_T(lambda b: m_sb[:, b, :], lambda b: ["m_sb"], 2, 2)
        for i in range(2):
            pan, pk = panel_next("kv%d" % i)
            for c in range(4):
                bank, bk = nb()
                mm(bank[:, 0:NMEM], [(pan[:, kc, c * 128:(c + 1) * 128], hT[:, kc, 0:NMEM]) for kc in range(KC)],
                   hTk + [pk], [bk])
                act(lambda e, i=i, c=c, bank=bank: e.activation(out=KT[:, i * 4 + c, :], in_=bank[:, 0:NMEM],
                                                                func=AF.Copy), [bk], [("KT", i * 4 + c)])
            panel_load()
        for i in range(2):
            pan, pk = panel_next("kv%d" % (2 + i))
            for mb in range(2):
                bank, bk = nb()
                mm(bank[:, :], [(hT[:, kc, mb * 128:(mb + 1) * 128], pan[:, kc, :]) for kc in range(KC)],
                   hTk + [pk], [bk])
                act(lambda e, i=i, mb=mb, bank=bank: e.activation(out=Vm[:, mb, i * 512:(i + 1) * 512],
                                                                  in_=bank[:, :], func=AF.Copy),
                    [bk], [("Vm", mb)])
            panel_load()
        S.mute = False
        dve(lambda e: e.memset(Sst[:], 0.0), [], ["Sst"])
        dve(lambda e: e.memset(pext[:, :, 0:16], 0.0), [], ["pext_h"])
        dve(lambda e: e.memset(halo[:], 0.0), [], ["halo"] + [("halo", q) for q in range(NCH)])

        for t in range(ntile):
            xb = xbuf[tile_idx % 2]
            xk = "x%d" % (tile_idx % 2)
            row0 = s * SEQ + t * T
            xkeys = [(xk, b) for b in range(NB)]
            if t + 1 < ntile:
                load_x(s, t + 1)
            elif s + 1 < nseq:
                load_x(s + 1, 0)
                load_mem(s + 1)

            S.mute = en is not None and "mix" not in en
            norm_x(xb, xk, 0)
            bank, bk = nb()
            mm(bank[0:16, :], [(wg_b[:, kc, :], hT[:, kc, :]) for kc in range(KC)], hTk + ["wg_b"], [bk])
            act(lambda e, bank=bank: e.activation(out=gT[0:16, :], in_=bank[0:16, :], func=AF.Copy), [bk], ["gT"])
            for b in range(NB):
                if b % 2 == 0:
                    zb, zk = nb()
                zo = zb[:, (b % 2) * 256:(b % 2 + 1) * 256]
                mm(zo, [(gT[0:17, b * 128:(b + 1) * 128], gkw[0:17, :])], ["gT", "gkw"], [zk])
                act(lambda e, b=b, zo=zo: e.activation(out=lg.ap[:, b, :], in_=zo, func=AF.Exp, scale=-1.0),
                    [zk], lg.kc(b))
                act(lambda e, b=b: e.activation(out=lg.ap[:, b, :], in_=lg.ap[:, b, :], func=AF.Ln, bias=1.0),
                    lg.kc(b), lg.kc(b))
            ck(1)
            pan, pk = panel_next("in_p")
            for c in range(4):
                bank, bk = nb()
                mm(bank[:, :], [(pan[:, kc, c * 128:(c + 1) * 128], hT[:, kc, :]) for kc in range(KC)],
                   hTk + [pk], [bk])
                act(lambda e, c=c, bank=bank: e.activation(out=pext[:, c, 16:16 + T], in_=bank[:, :], func=AF.Copy),
                    [bk], [("pext", c)])
            panel_load()
            ck(2)
            GT = []
            for pr in range(2):
                bank, bk = nb()
                mm_multi([(bank[:, b * 128:(b + 1) * 128], [(lg.ap[:, b, pr * 128:(pr + 1) * 128], tri[:, :])])
                          for b in range(NB)], lg.k() + ["tri"], [bk])
                GT.append((bank, bk))
                act(lambda e, pr=pr, bank=bank: e.activation(out=eG.ap[:, pr, :], in_=bank[:, :], func=AF.Exp),
                    [bk], eG.kc(pr))
                act(lambda e, pr=pr, bank=bank: e.activation(out=enG.ap[:, pr, :], in_=bank[:, :], func=AF.Exp,
                                                             scale=-1.0), [bk], enG.kc(pr))
                act(lambda e, pr=pr, bank=bank: e.activation(
                    out=dec[:, pr, :], in_=bank[:, :].rearrange("p (c j) -> p c j", j=64)[:, :, 63], func=AF.Exp),
                    [bk], [("dec", pr)])
            eDk = []
            for b in range(NB):
                if b % 2 == 0:
                    db, dk_ = nb()
                do = db[:, (b % 2) * 256:(b % 2 + 1) * 256]
                mm(do, [(tris[:, :], lg.ap[:, b, :])], lg.kc(b) + ["tris"], [dk_])
                act(lambda e, do=do, b=b: e.activation(out=eD.ap[:, b, :], in_=do, func=AF.Exp), [dk_], eD.kc(b))
            ck(3)
            pan, pk = panel_next("in_qk")
            for c in range(4):
                bank, bk = nb()
                mm(bank[:, :], [(pan[:, kc, c * 128:(c + 1) * 128], hT[:, kc, :]) for kc in range(KC)],
                   hTk + [pk], [bk])
                if c < 2:
                    dve(lambda e, c=c, bank=bank: e.scalar_tensor_tensor(
                        out=qdec.ap[:, c, :], in0=bank[:, :], scalar=0.125, in1=eG.ap[:, c, :],
                        op0=ALU.mult, op1=ALU.mult), [bk] + eG.kc(c), qdec.kc(c))
                else:
                    dve(lambda e, c=c, bank=bank: e.tensor_tensor(
                        out=kdec.ap[:, c - 2, :], in0=bank[:, :], in1=enG.ap[:, c - 2, :], op=ALU.mult),
                        [bk] + enG.kc(c - 2), kdec.kc(c - 2))
            for b in range(NB):
                if b % 2 == 0:
                    kb_, kk_ = nb()
                ko = kb_[:, (b % 2) * 256:(b % 2 + 1) * 256]
                mm(ko, [(hT[:, kc, b * 128:(b + 1) * 128], pan[:, kc, 256:512]) for kc in range(KC)],
                   hTk + [pk], [kk_])
                dve(lambda e, b=b, ko=ko: e.tensor_tensor(out=kte.ap[:, b, :], in0=ko, in1=eD.ap[:, b, :],
                                                          op=ALU.mult), [kk_] + eD.kc(b), kte.kc(b))
            panel_load()
            ck(4)
            pan, pk = panel_next("in_v")
            for b in range(NB):
                bank, bk = nb()
                mm(bank[:, :], [(hT[:, kc, b * 128:(b + 1) * 128], pan[:, kc, :]) for kc in range(KC)],
                   hTk + [pk], [bk])
                act(lambda e, b=b, bank=bank: e.activation(out=vtok.ap[:, b, :], in_=bank[:, :], func=AF.Copy),
                    [bk], vtok.kc(b))
            panel_load()
            ck(5)
            pextk = [("pext", c) for c in range(4)]
            for g in range(4):
                src_ap = pext[:, g, :]
                src_k = [("pext", g), "pext_h"]
                L = 16 + T
                sh = 1
                for it in range(g + 1):
                    dst = pa[it % 2]
                    dve(lambda e, src_ap=src_ap, dst=dst, sh=sh, L=L: e.tensor_tensor(
                        out=dst.ap[:, 2 * sh - 1:L], in0=src_ap[:, 2 * sh - 1:L], in1=src_ap[:, sh - 1:L - sh],
                        op=ALU.add),
                        src_k, dst.k())
                    src_ap = dst.ap
                    src_k = dst.k()
                    sh *= 2
                w = 2 ** (g + 1)
                dve(lambda e, g=g, src_ap=src_ap, w=w: e.scalar_tensor_tensor(
                    out=pooled.ap[:, g, :], in0=src_ap[:, 16:16 + T], scalar=1.0 / w, in1=pext[:, g, 16:16 + T],
                    op0=ALU.mult, op1=ALU.subtract), src_k + [("pext", g)], pooled.kc(g))
                if t == 0:
                    dve(lambda e, g=g, src_ap=src_ap: e.tensor_tensor(
                        out=src_ap[:, 16:32], in0=src_ap[:, 16:32], in1=invc[:, g, :], op=ALU.mult),
                        src_k + ["invc"], src_k)
                    dve(lambda e, g=g, src_ap=src_ap: e.tensor_tensor(
                        out=pooled.ap[:, g, 0:16], in0=src_ap[:, 16:32], in1=pext[:, g, 16:32], op=ALU.subtract),
                        src_k + [("pext", g)], pooled.kc(g))
                bank, bk = nb()
                mm(bank[:, :], [(poolw_b[:, g, :], pooled.ap[:, g, :])], pooled.kc(g) + ["poolw_b"], [bk])
                act(lambda e, g=g, bank=bank: e.activation(out=catT.ap[:, g, :], in_=bank[:, :], func=AF.Copy,
                                                           scale=pscale[:, g:g + 1]), [bk, "pscale"], catT.kc(g))
            dve(lambda e: e.tensor_copy(out=pext[:, :, 0:16], in_=pext[:, :, T:T + 16]), pextk + ["pext_h"],
                ["pext_h"])
            ck(7)
            for hh in range(2):
                for hl in range(2):
                    scb, sck = nb()
                    groups = []
                    for bl in range(2):
                        b = 2 * hh + bl
                        for hf in range(2):
                            c = 2 * b + hf
                            for hp in range(2):
                                groups.append((scb[64 * hf:64 * hf + 64, bl * 128 + hp * 64: bl * 128 + hp * 64 + 64],
                                               [(kdec.ap[64 * hl:64 * hl + 64, hp, c * 64:(c + 1) * 64],
                                                 qdec.ap[64 * hl:64 * hl + 64, hp, c * 64:(c + 1) * 64])]))
                    mm_multi(groups, kdec.k() + qdec.k(), [sck])
                    dve(lambda e, hh=hh, hl=hl, scb=scb: e.tensor_tensor(
                        out=scm.ap[:, 2 * hh:2 * hh + 2, hl * 128:(hl + 1) * 128],
                        in0=scb[:, 0:256].rearrange("p (a b) -> p a b", a=2),
                        in1=cmask2[:, :, :],
                        op=ALU.mult), [sck, "cmask"], scm.kc(2 * hh, 2 * hh + 2))
                ck(8)
                kvb = []
                for hf in range(2):
                    bank, bk = nb()
                    groups = []
                    for bl in range(2):
                        b = 2 * hh + bl
                        for h in range(4):
                            pr, hl = divmod(h, 2)
                            groups.append((bank[64 * hl:64 * hl + 64, bl * 256 + pr * 128: bl * 256 + pr * 128 + 128],
                                           [(kte.ap[64 * hf:64 * hf + 64, b, h * 64:(h + 1) * 64],
                                             vtok.ap[64 * hf:64 * hf + 64, b, h * 128:(h + 1) * 128])]))
                    mm_multi(groups, kte.kc(2 * hh, 2 * hh + 2) + vtok.kc(2 * hh, 2 * hh + 2), [bk])
                    kvb.append((bank, bk))
                ck(9)
                for cl in range(4):
                    c = 4 * hh + cl
                    bl, hf = divmod(cl, 2)
                    bank, bk = kvb[hf]
                    act(lambda e, c=c: e.activation(out=Sbf.ap[:, c, :], in_=Sst[:].rearrange("p a b -> p (a b)"),
                                                    func=AF.Copy), ["Sst"], Sbf.kc(c))
                    for pr in range(2):
                        dve(lambda e, c=c, pr=pr, bank=bank, bl=bl: e.scalar_tensor_tensor(
                            out=Sst[:, pr, :], in0=Sst[:, pr, :], scalar=dec[:, pr, c:c + 1],
                            in1=bank[:, bl * 256 + pr * 128: bl * 256 + pr * 128 + 128],
                            op0=ALU.mult, op1=ALU.add), ["Sst", bk, ("dec", pr)], ["Sst"])
            ck(6)
            pan, pk = panel_next("in_r")
            for c in range(4):
                bank, bk = nb()
                mm(bank[:, :], [(pan[:, kc, c * 128:(c + 1) * 128], hT[:, kc, :]) for kc in range(KC)],
                   hTk + [pk], [bk])
                act(lambda e, c=c, bank=bank: e.activation(out=sr.ap[:, c, :], in_=bank[:, :], func=AF.Silu),
                    [bk], sr.kc(c))
            panel_load()
            for hh in range(2):
                ck(10)
                oib = []
                for hf in range(2):
                    bank, bk = nb()
                    groups = []
                    for bl in range(2):
                        b = 2 * hh + bl
                        for h in range(4):
                            hp, hl = divmod(h, 2)
                            groups.append((bank[:, bl * 256 + h * 64: bl * 256 + h * 64 + 64],
                                           [(vtok.ap[64 * hf:64 * hf + 64, b, h * 128:(h + 1) * 128],
                                             scm.ap[64 * hf:64 * hf + 64, b, (hl * 2 + hp) * 64:(hl * 2 + hp) * 64 + 64])]))
                    mm_multi(groups, vtok.kc(2 * hh, 2 * hh + 2) + scm.kc(2 * hh, 2 * hh + 2), [bk])
                    oib.append((bank, bk))
                ck(11)
                for hl in range(2):
                    bank, bk = nb()
                    groups = []
                    for cl in range(4):
                        c = 4 * hh + cl
                        for hp in range(2):
                            groups.append((bank[:, cl * 128 + hp * 64: cl * 128 + hp * 64 + 64],
                                           [(Sbf.ap[64 * hl:64 * hl + 64, c, hp * 128:(hp + 1) * 128],
                                             qdec.ap[64 * hl:64 * hl + 64, hp, c * 64:(c + 1) * 64])]))
                    mm_multi(groups, Sbf.kc(4 * hh, 4 * hh + 4) + qdec.k(), [bk])
                    for hp in range(2):
                        act(lambda e, hl=hl, hp=hp, hh=hh, bank=bank: e.activation(
                            out=o_sb.ap[:, 2 * hp + hl, hh * 256:(hh + 1) * 256].rearrange("p (c i) -> p c i", i=64),
                            in_=bank[:, :].rearrange("p (c q i) -> p c q i", q=2, i=64)[:, :, hp, :],
                            func=AF.Copy), [bk], o_sb.kc(2 * hp + hl))
                ck(12)
                for hf in range(2):
                    bank, bk = oib[hf]
                    for bl in range(2):
                        cl = 2 * bl + hf
                        dve(lambda e, hh=hh, bl=bl, cl=cl, bank=bank: e.tensor_tensor(
                            out=o_sb.ap[:, :, hh * 256 + cl * 64: hh * 256 + cl * 64 + 64],
                            in0=o_sb.ap[:, :, hh * 256 + cl * 64: hh * 256 + cl * 64 + 64],
                            in1=bank[:, bl * 256:(bl + 1) * 256].rearrange("p (h i) -> p h i", i=64),
                            op=ALU.add), [bk] + o_sb.k(), o_sb.k())
            for h in range(4):
                act(lambda e, h=h: e.activation(out=sq.ap[:, h, :], in_=o_sb.ap[:, h, :], func=AF.Square),
                    o_sb.kc(h), sq.kc(h))
            ck(13)
            for h in range(4):
                bank, bk = nb()
                mm(bank[:, :], [(ones_f[:, :], sq.ap[:, h, :])], sq.kc(h) + ["ones_f"], [bk])
                r_ = rb[h % 2]
                act(lambda e, bank=bank, r_=r_: e.activation(out=r_.ap, in_=bank[:, :], func=AF.Ln,
                                                             bias=epsc[:, 0:1]), [bk, "epsc"], r_.k())
                act(lambda e, r_=r_: e.activation(out=r_.ap, in_=r_.ap, func=AF.Exp, scale=-0.5), r_.k(), r_.k())
                dve(lambda e, h=h, r_=r_: e.scalar_tensor_tensor(
                    out=gl.ap, in0=o_sb.ap[:, h, :], scalar=gnw[:, 0:1], in1=r_.ap, op0=ALU.mult, op1=ALU.mult),
                    o_sb.kc(h) + r_.k() + ["gnw"], gl.k())
                dve(lambda e, h=h: e.tensor_tensor(out=catT.ap[:, 4 + h, :], in0=gl.ap, in1=sr.ap[:, h, :],
                                                   op=ALU.mult), gl.k() + sr.kc(h), catT.kc(4 + h))
            ck(14)
            proj_residual(catT, ["wout0", "wout1"], xb, xk)

            S.mute = en is not None and "xat" not in en
            norm_x(xb, xk, 1)
            for i in range(2):
                pan, pk = panel_next("wq%d" % i)
                for c in range(4):
                    bank, bk = nb()
                    mm(bank[:, :], [(pan[:, kc, c * 128:(c + 1) * 128], hT[:, kc, :]) for kc in range(KC)],
                       hTk + [pk], [bk])
                    cc = i * 4 + c
                    if c % 2 == 0:
                        act(lambda e, cc=cc, bank=bank: e.activation(out=qxT.ap[:, cc, :], in_=bank[:, :],
                                                                     func=AF.Copy), [bk], qxT.kc(cc))
                    else:
                        dve(lambda e, cc=cc, bank=bank: e.tensor_copy(out=qxT.ap[:, cc, :], in_=bank[:, :]),
                            [bk], qxT.kc(cc))
                panel_load()
            for h in range(4):
                for mc in range(2):
                    bank, bk = nb()
                    mm(bank[:, :], [(KT[:, 2 * h + dc, mc * 128:(mc + 1) * 128], qxT.ap[:, 2 * h + dc, :])
                                    for dc in range(2)],
                       qxT.kc(2 * h, 2 * h + 2) + [("KT", 2 * h), ("KT", 2 * h + 1)], [bk])
                    act(lambda e, h=h, mc=mc, bank=bank: e.activation(out=PT.ap[:, 2 * h + mc, :], in_=bank[:, :],
                                                                      func=AF.Exp, scale=1.0 / 16.0),
                        [bk], PT.kc(2 * h + mc))
                smb, smk = nb()
                mm(smb[:, :], [(ones_b[:, :], PT.ap[:, 2 * h + mc, :]) for mc in range(2)],
                   PT.kc(2 * h, 2 * h + 2) + ["ones_b"], [smk])
                r_ = rs[h % 2]
                dve(lambda e, smb=smb, r_=r_: e.reciprocal(out=r_.ap, in_=smb[:, :]), [smk], r_.k())
                for dc in range(2):
                    bank, bk = nb()
                    mm(bank[:, :], [(Vm[:, mc, h * 256 + dc * 128: h * 256 + dc * 128 + 128], PT.ap[:, 2 * h + mc, :])
                                    for mc in range(2)],
                       PT.kc(2 * h, 2 * h + 2) + [("Vm", 0), ("Vm", 1)], [bk])
                    dve(lambda e, h=h, dc=dc, bank=bank, r_=r_: e.tensor_tensor(
                        out=oxT.ap[:, 2 * h + dc, :], in0=bank[:, :], in1=r_.ap, op=ALU.mult),
                        [bk] + r_.k(), oxT.kc(2 * h + dc))
            proj_residual(oxT, ["wo0", "wo1"], xb, xk)

            S.mute = en is not None and "ffn" not in en
            norm_x(xb, xk, 3)
            for j in range(11):
                pan, pk = panel_next("up%d" % j)
                ys = []
                for r4 in range(4):
                    q = 4 * j + r4
                    bank, bk = nb()
                    mm(bank[:, :], [(pan[:, kc, r4 * 128:(r4 + 1) * 128], hT[:, kc, :]) for kc in range(KC)],
                       hTk + [pk], [bk])
                    ub = ubuf[q % 4]
                    yb = ybuf[q % 4]
                    act(lambda e, q=q, ub=ub: e.activation(out=ub.ap[:, 0:2], in_=halo[:, q, :], func=AF.Copy),
                        [("halo", q), "halo"], ub.k())
                    act(lambda e, ub=ub, bank=bank: e.activation(out=ub.ap[:, 2:2 + T], in_=bank[:, :], func=AF.Copy),
                        [bk], ub.k())
                    act(lambda e, q=q, yb=yb, bank=bank: e.activation(
                        out=yb.ap, in_=bank[:, :], func=AF.Identity, scale=cw[:, q, 2:3], bias=cb[:, q:q + 1]),
                        [bk, "cw", "cb"], yb.k())
                    act(lambda e, q=q, bank=bank: e.activation(out=halo[:, q, :], in_=bank[:, T - 2:T], func=AF.Copy),
                        [bk], [("halo", q)])
                    dve(lambda e, q=q, ub=ub, yb=yb: e.scalar_tensor_tensor(
                        out=yb.ap, in0=ub.ap[:, 1:1 + T], scalar=cw[:, q, 1:2], in1=yb.ap,
                        op0=ALU.mult, op1=ALU.add), ub.k() + yb.k() + ["cw"], yb.k())
                    dve(lambda e, q=q, ub=ub, yb=yb: e.scalar_tensor_tensor(
                        out=yb.ap, in0=ub.ap[:, 0:T], scalar=cw[:, q, 0:1], in1=yb.ap,
                        op0=ALU.mult, op1=ALU.add), ub.k() + yb.k() + ["cw"], yb.k())
                    ys.append(yb)
                panel_load()
                for r2 in range(2):
                    ch = 2 * j + r2
                    sgb = sg[ch % 2]
                    yg, yv = ys[r2], ys[2 + r2]
                    act(lambda e, yg=yg, sgb=sgb: e.activation(out=sgb.ap, in_=yg.ap, func=AF.Silu),
                        yg.k(), sgb.k())
                    dve(lambda e, ch=ch, sgb=sgb, yv=yv: e.tensor_tensor(out=actT.ap[:, ch, :], in0=sgb.ap,
                                                                         in1=yv.ap, op=ALU.mult),
                        sgb.k() + yv.k(), actT.kc(ch))
            for nh in range(2):
                banks = [nb() for _ in range(NB)]
                for kg in range(3):
                    pan, pk = panel_next("dn%d_%d" % (nh, kg))
                    nk = 8 if kg < 2 else 6
                    for b in range(NB):
                        bank, bk = banks[b]

                        def fn(e, b=b, bank=bank, kg=kg, nk=nk, pan=pan):
                            ins = None
                            for kc in range(nk):
                                ins = e.matmul(bank[:, :], actT.ap[:, kg * 8 + kc, b * 128:(b + 1) * 128],
                                               pan[:, kc, :], start=(kg == 0 and kc == 0),
                                               stop=(kg == 2 and kc == nk - 1))
                            return ins
                        S.op("pe", fn, actT.kc(kg * 8, kg * 8 + nk) + [pk], [bk])
                    panel_load()
                for b in range(NB):
                    bank, bk = banks[b]
                    dve(lambda e, b=b, nh=nh, bank=bank, xb=xb: e.tensor_tensor(
                        out=xb[:, b, nh * 512:(nh + 1) * 512], in0=xb[:, b, nh * 512:(nh + 1) * 512],
                        in1=bank[:, :], op=ALU.add), [bk, (xk, b)], [(xk, b)])

            S.mute = en is not None and "fin" not in en
            for b in range(NB):
                act(lambda e, b=b, xb=xb: e.activation(out=junk[:], in_=xb[:, b, :], func=AF.Square,
                                                accum_out=ss[:, b:b + 1]), [(xk, b)], ["junk", ("ss", b)])
            act(lambda e: e.activation(out=rstd[:, :], in_=ss[:, :], func=AF.Ln, scale=1.0 / D, bias=epsc[:, 0:1]),
                [("ss", b) for b in range(NB)] + ["epsc"], [("rstd", b_) for b_ in range(4)])
            act(lambda e: e.activation(out=rstd[:, :], in_=rstd[:, :], func=AF.Exp, scale=-0.5),
                [("rstd", b_) for b_ in range(4)], [("rstd", b_) for b_ in range(4)])
            for b in range(NB):
                dve(lambda e, b=b, xb=xb: e.scalar_tensor_tensor(out=xb[:, b, :], in0=xb[:, b, :], scalar=rstd[:, b:b + 1],
                                                          in1=wf_bc[:, :], op0=ALU.mult, op1=ALU.mult),
                    [(xk, b), ("rstd", b), "wf_bc"], [(xk, b)])
            S.mute = False
            S.dma("sp", xk, lambda e, xb=xb, row0=row0: e.dma_start(
                out=out_d.ap()[row0:row0 + T, :].rearrange("(b p) d -> p b d", p=128), in_=xb[:]),
                reads=xkeys, writes=[("out", tile_idx)])
            tile_idx += 1

    S.wait_all("sp", [S.lastw[("out", i)] for i in range(tile_idx)])
    S.emit()
    return nc


def _prep_shared(inp):
    f = lambda a: np.ascontiguousarray(np.asarray(a, dtype=np.float32))
    w_in = f(inp["w_in"])[0]
    tri, tris, cmask, ident, invc = _consts()
    col = lambda v, n: np.ascontiguousarray(f(v).reshape(n, 128).T)
    nw = np.stack([col(inp["norm_mix_w"][0], 8), col(inp["norm_xattn_w"][0], 8),
                   col(inp["norm_mem_w"][0], 8), col(inp["norm_ffn_w"][0], 8)], axis=1)
    wg = w_in[:, 1536:1552].reshape(8, 128, 16).transpose(1, 0, 2)
    poolw = f(inp["pool_w"])[0].transpose(1, 0, 2)
    gkw = np.concatenate([f(inp["gk_w2"])[0], f(inp["gk_b"])[0][None, :]], axis=0)
    qcols = np.array([_chunk_col(q) for q in range(NCH)])
    idx = qcols[:, None] + np.arange(128)[None, :]
    cwf = f(inp["ffn_conv_w"])[0]
    cw = cwf[:, idx].transpose(2, 1, 0)
    cb = f(inp["ffn_conv_b"])[0][idx].T
    shared = {
        "wpan": _pack_weights(w_in, f(inp["w_out"])[0], f(inp["xattn_wq"])[0], f(inp["xattn_wkv"])[0],
                              f(inp["xattn_wo"])[0], f(inp["ffn_w_up"])[0], f(inp["ffn_w_down"])[0]),
        "wg": np.ascontiguousarray(wg).reshape(128, 128),
        "nw": np.ascontiguousarray(nw).reshape(128, 32),
        "wf": f(inp["norm_final_w"]).reshape(1, D),
        "poolw": np.ascontiguousarray(poolw).reshape(128, 512),
        "pscale": col(inp["pool_scale"][0], 4),
        "gkw": np.ascontiguousarray(gkw),
        "gnw": f(inp["gla_norm_w"])[0].reshape(128, 1),
        "cw": np.ascontiguousarray(cw).reshape(128, NCH * 3),
        "cb": np.ascontiguousarray(cb),
        "tri": tri, "tris": tris, "cmask": cmask, "ident": ident, "invc": invc,
    }
    return shared


def kernel(**inputs):
    x = np.asarray(inputs["x"], dtype=np.float32)
    mem = np.asarray(inputs["mem"], dtype=np.float32)
    shared = _prep_shared(inputs)
    nc = build_program()
    in_maps = []
    for c in range(NCORES):
        m = dict(shared)
        m["x"] = np.ascontiguousarray(x[c * SEQ_PER_CORE:(c + 1) * SEQ_PER_CORE]).reshape(SEQ_PER_CORE * SEQ, D)
        m["mem"] = np.ascontiguousarray(mem[c * SEQ_PER_CORE:(c + 1) * SEQ_PER_CORE]).reshape(SEQ_PER_CORE * NMEM, D)
        in_maps.append(m)
    res = run_bass_kernel_spmd(nc, in_maps, core_ids=list(range(NCORES)))
    out = np.concatenate([r["out"].reshape(SEQ_PER_CORE, SEQ, D) for r in res.results], axis=0)
    return out.astype(np.float32)
```

```python
import contextlib
import numpy as np
import concourse.bass as bass
import concourse.mybir as mybir
from concourse.bass_utils import run_bass_kernel_spmd

F32 = mybir.dt.float32
BF16 = mybir.dt.bfloat16
AF = mybir.ActivationFunctionType
ALU = mybir.AluOpType

D = 1024
SEQ = 2048
NMEM = 256
T = 512
NB = 4
KC = 8
DFF = 2816
NCH = 44
EPS = 1e-6
NCORES = 8
SEQ_PER_CORE = 4
RING = 4
NCV = 2
STRICT = True


class Sched:
    ENGS = ("pe", "act", "dve", "pool", "sp")

    def __init__(self, nc):
        self.nc = nc
        self.ops = {e: [] for e in self.ENGS}
        self.cnt = {}
        self.seen = {e: {} for e in self.ENGS}
        self.lastw = {}
        self.readers = {}
        self.chan_last = {}
        self.mute = False

    def _deps(self, reads, writes, eng=None):
        deps = []
        own = ("c", eng)
        for b in reads:
            t = self.lastw.get(b)
            if t is not None:
                deps.append(t)
            if isinstance(b, tuple) and b[0] in ("ps", "pt"):
                r = self.readers.get(b)
                if r:
                    deps.extend((k, v) for k, v in r.items() if k != own)
        for b in writes:
            t = self.lastw.get(b)
            if t is not None and (STRICT or t[0] != own):
                deps.append(t)
            r = self.readers.get(b)
            if r:
                deps.extend((k, v) for k, v in r.items() if (STRICT or k != own))
        return deps

    def _commit(self, tok, reads, writes):
        k, v = tok
        for b in reads:
            d = self.readers.setdefault(b, {})
            if d.get(k, 0) < v:
                d[k] = v
        for b in writes:
            self.lastw[b] = tok
            self.readers[b] = {}

    def _waits(self, eng, deps):
        need = {}
        for (k, v) in deps:
            if eng == "pe" and k == ("c", "pe"):
                continue
            if need.get(k, 0) < v:
                need[k] = v
        out = []
        seen = self.seen[eng]
        for k, v in need.items():
            if seen.get(k, 0) < v:
                seen[k] = v
                out.append((k, v))
        return out

    def op(self, eng, fn, reads=(), writes=()):
        if self.mute:
            return None
        waits = self._waits(eng, self._deps(reads, writes, eng))
        k = ("c", eng)
        self.cnt[k] = self.cnt.get(k, 0) + 1
        tok = (k, self.cnt[k])
        self._commit(tok, reads, writes)
        self.ops[eng].append((waits, fn, k, 1))
        return tok

    def dma(self, eng, chan, fn, reads=(), writes=()):
        if self.mute:
            return None
        deps = self._deps(reads, writes)
        k = ("d", chan)
        if k in self.chan_last:
            deps.append(self.chan_last[k])
        waits = self._waits(eng, deps)
        self.cnt[k] = self.cnt.get(k, 0) + 16
        tok = (k, self.cnt[k])
        self.chan_last[k] = tok
        self._commit(tok, reads, writes)
        self.ops[eng].append((waits, fn, k, 16))
        return tok

    def wait_all(self, eng, toks):
        waits = self._waits(eng, [t for t in toks if t is not None])
        self.ops[eng].append((waits, None, None, 0))

    def emit(self):
        nc = self.nc
        sems = {}
        with contextlib.ExitStack() as st:
            for k in self.cnt:
                sems[k] = st.enter_context(nc.semaphore("s_%s_%s" % k))
            block = st.enter_context(nc.Block())

            def run(engobj, e):
                for (waits, fn, semk, incv) in self.ops[e]:
                    for (k, v) in waits:
                        engobj.wait_ge(sems[k], v)
                    if fn is not None:
                        ins = fn(engobj)
                        ins.then_inc(sems[semk], incv)

            @block.tensor
            def _(eng):
                run(eng, "pe")

            @block.scalar
            def _(eng):
                run(eng, "act")

            @block.vector
            def _(eng):
                run(eng, "dve")

            @block.gpsimd
            def _(eng):
                run(eng, "pool")

            @block.sync
            def _(eng):
                run(eng, "sp")


def _chunk_col(q):
    j, r = divmod(q, 4)
    return (2 * j + (r % 2)) * 128 + (DFF if r >= 2 else 0)


def _panel(W, r0, nk, cols):
    P = np.zeros((128, KC, 512), np.float32)
    blk = W[r0:r0 + nk * 128][:, cols]
    P[:, :nk, :] = blk.reshape(nk, 128, 512).transpose(1, 0, 2)
    return P.reshape(128, KC * 512)


PANEL_NAMES = (["kv%d" % i for i in range(4)] + ["in_p", "in_qk", "in_v", "in_r", "wout0", "wout1",
               "wq0", "wq1", "wo0", "wo1"] + ["up%d" % i for i in range(11)] +
               ["dn%d_%d" % (nh, kg) for nh in range(2) for kg in range(3)])
PID = {n: i for i, n in enumerate(PANEL_NAMES)}
NPAN = len(PANEL_NAMES)
TILE_PANELS = (["in_p", "in_qk", "in_v", "in_r", "wout0", "wout1", "wq0", "wq1", "wo0", "wo1"] +
               ["up%d" % i for i in range(11)] + ["dn%d_%d" % (nh, kg) for nh in range(2) for kg in range(3)])


def _pack_weights(w_in, w_out, wq, wkv, wo, w_up, w_down):
    ar = np.arange(512)
    out = np.zeros((NPAN, 128, KC * 512), np.float32)
    for i in range(4):
        out[PID["kv%d" % i]] = _panel(wkv, 0, 8, ar + 512 * i)
    out[PID["in_p"]] = _panel(w_in, 0, 8, ar)
    out[PID["in_qk"]] = _panel(w_in, 0, 8, ar + 512)
    out[PID["in_v"]] = _panel(w_in, 0, 8, ar + 1024)
    out[PID["in_r"]] = _panel(w_in, 0, 8, ar + 1552)
    for i in range(2):
        out[PID["wout%d" % i]] = _panel(w_out, 0, 8, ar + 512 * i)
        out[PID["wq%d" % i]] = _panel(wq, 0, 8, ar + 512 * i)
        out[PID["wo%d" % i]] = _panel(wo, 0, 8, ar + 512 * i)
    for j in range(11):
        cols = np.concatenate([np.arange(128) + _chunk_col(4 * j + r) for r in range(4)])
        out[PID["up%d" % j]] = _panel(w_up, 0, 8, cols)
    for nh in range(2):
        for kg in range(3):
            nk = 8 if kg < 2 else 6
            out[PID["dn%d_%d" % (nh, kg)]] = _panel(w_down, kg * 1024, nk, ar + 512 * nh)
    return out


def _consts():
    s = np.arange(128)[:, None]
    t = np.arange(128)[None, :]
    same = (s // 64) == (t // 64)
    tri = np.where(same & (s <= t), -1.0 / 16.0, 0.0).astype(np.float32)
    tris = np.where(same & (s > t), -1.0 / 16.0, 0.0).astype(np.float32)
    j = (np.arange(128) % 64)[:, None, None]
    i = np.arange(64)[None, None, :]
    cmask = np.broadcast_to((j <= i), (128, 4, 64)).astype(np.float32).reshape(128, 256)
    ident = np.eye(128, dtype=np.float32)
    invc = np.zeros((128, 4, 16), np.float32)
    tt = np.arange(16)
    for g, w in enumerate((2, 4, 8, 16)):
        invc[:, g, :] = 1.0 / np.minimum(tt + 1, w)
    return tri, tris, cmask, ident, invc.reshape(128, 64)


def build_program(nseq=SEQ_PER_CORE, ntile=SEQ // T, dbg=None):
    nc = bass.Bass("TRN2", target_bir_lowering=False)
    S = Sched(nc)
    en = dbg
    import os as _os
    MIXSTOP = int(_os.environ.get("MIXSTOP", "99"))

    def ck(n):
        if n > MIXSTOP:
            S.mute = True

    def din(name, shape):
        return nc.dram_tensor(name, list(shape), F32, kind="ExternalInput")

    x_d = din("x", [nseq * SEQ, D])
    mem_d = din("mem", [nseq * NMEM, D])
    wpan_d = din("wpan", [NPAN, 128, KC * 512])
    wg_d = din("wg", [128, KC * 16])
    nw_d = din("nw", [128, 4 * KC])
    wf_d = din("wf", [1, D])
    poolw_d = din("poolw", [128, 4 * 128])
    pscale_d = din("pscale", [128, 4])
    gkw_d = din("gkw", [17, 256])
    gnw_d = din("gnw", [128, 1])
    cw_d = din("cw", [128, NCH * 3])
    cb_d = din("cb", [128, NCH])
    tri_d = din("tri", [128, 128])
    tris_d = din("tris", [128, 128])
    cmask_d = din("cmask", [128, 256])
    ident_d = din("ident", [128, 128])
    invc_d = din("invc", [128, 64])
    out_d = nc.dram_tensor("out", [nseq * SEQ, D], F32, kind="ExternalOutput")
    wbf_d = nc.dram_tensor("wbf", [NPAN, 128, KC * 512], BF16, kind="Internal")

    def sb(name, shape, dt=F32):
        return nc.alloc_sbuf_tensor("s_" + name, list(shape), dt)

    xbuf = [sb("xb%d" % i, [128, NB, D]) for i in range(2)]
    ring = [sb("ring%d" % i, [128, KC, 512], BF16) for i in range(RING)]
    hT = sb("hT", [128, KC, T], BF16)
    hb = sb("hb", [128, NB, D], BF16)
    junk = sb("junk", [128, D], BF16)
    ss = sb("ss", [128, 4])
    rstd = sb("rstd", [128, 4])
    epsc = sb("epsc", [128, 1])
    wf_bc = sb("wf_bc", [128, D])
    nw = sb("nw", [128, 4, KC])
    wg_f = sb("wg_f", [128, KC, 16])
    wg_b = sb("wg_b", [128, KC, 16], BF16)
    poolw_f = sb("poolw_f", [128, 4, 128])
    poolw_b = sb("poolw_b", [128, 4, 128], BF16)
    pscale = sb("pscale", [128, 4])
    gkw = sb("gkw", [17, 256])
    gnw = sb("gnw", [128, 1])
    cw = sb("cw", [128, NCH, 3])
    cb = sb("cb", [128, NCH])
    tri = sb("tri", [128, 128])
    tris = sb("tris", [128, 128])
    cmask = sb("cmask", [128, 4, 64])
    cmask2 = cmask[:].rearrange("p (a b) c -> p a (b c)", a=2)
    ident_f = sb("ident_f", [128, 128])
    ident_b = sb("ident_b", [128, 128], BF16)
    invc = sb("invc", [128, 4, 16])
    ones_f = sb("ones_f", [128, 128])
    ones_b = sb("ones_b", [128, 128], BF16)
    KT = sb("KT", [128, 8, NMEM], BF16)
    Vm = sb("Vm", [128, 2, D], BF16)
    pext = sb("pext", [128, 4, 16 + T])
    Sst = sb("Sst", [128, 2, 128])
    halo = sb("halo", [128, NCH, 2])
    dec = sb("dec", [128, 2, 8])
    gT = sb("gT", [17, T])
    m_sb = sb("m_sb", [128, 2, D])

    ARENA_KB = 80
    arena = sb("arena", [128, ARENA_KB * 256])

    class AV:
        def __init__(self, off_kb, shape, dt):
            esz = 4 if dt == F32 else 2
            n = int(np.prod(shape))
            self.off = off_kb * 1024
            self.esz = esz
            self.n = n
            assert self.off + n * esz <= ARENA_KB * 1024, (off_kb, shape)
            a = arena[:, self.off // 4:(self.off + n * esz + 3) // 4]
            if dt != F32:
                a = a.bitcast(dt)
            if len(shape) == 2:
                a = a.rearrange("p (a b) -> p a b", a=shape[0])
            elif len(shape) == 3:
                a = a.rearrange("p (a b c) -> p a b c", a=shape[0], b=shape[1])
            self.ap = a
            self.shape = shape

        def k(self, lo=0, hi=None):
            hi = self.n if hi is None else hi
            b0 = (self.off + lo * self.esz) // 1024
            b1 = (self.off + hi * self.esz - 1) // 1024
            return [("ar", i) for i in range(b0, b1 + 1)]

        def kc(self, i, j=None):
            per = self.n // self.shape[0]
            j = i + 1 if j is None else j
            return self.k(i * per, j * per)

    catT = AV(0, [8, T], BF16)
    o_sb = AV(8, [4, T], F32)
    sq = AV(16, [4, T], F32)
    rb = [AV(24 + 2 * i, [T], F32) for i in range(2)]
    gl = AV(28, [T], F32)
    lg = AV(30, [4, 256], F32)
    eD = AV(34, [4, 256], F32)
    eG = AV(38, [2, T], F32)
    enG = AV(42, [2, T], F32)
    qdec = AV(46, [2, T], BF16)
    kdec = AV(48, [2, T], BF16)
    kte = AV(50, [4, 256], BF16)
    vtok = AV(52, [4, T], BF16)
    scm = AV(56, [4, 256], BF16)
    sr = AV(58, [4, T], F32)
    pooled = AV(66, [4, T], BF16)
    pa = [AV(70 + 3 * i, [16 + T], F32) for i in range(2)]
    Sbf = AV(76, [8, 256], BF16)
    oxT = AV(0, [8, T], BF16)
    qxT = AV(8, [8, T], BF16)
    PT = AV(16, [8, T], BF16)
    rs = [AV(24 + 2 * i, [T], F32) for i in range(2)]
    actT = AV(8, [22, T], BF16)
    ubuf = [AV(30 + 3 * i, [2 + T], F32) for i in range(4)]
    ybuf = [AV(42 + 2 * i, [T], F32) for i in range(4)]
    sg = [AV(50 + 2 * i, [T], F32) for i in range(2)]

    NPS = 6
    psb = [nc.alloc_psum_tensor("ps%d" % i, [128, 512], F32) for i in range(NPS)]
    ptb = [nc.alloc_psum_tensor("pt%d" % i, [128, 1024], BF16) for i in range(2)]
    rr = {"ps": 0, "pt": 0}

    def nb():
        i = rr["ps"]
        rr["ps"] = (i + 1) % NPS
        return psb[i], ("ps", i)

    def npt():
        i = rr["pt"]
        rr["pt"] = (i + 1) % 2
        return ptb[i], ("pt", i)

    def act(fn, reads, writes):
        return S.op("act", fn, reads, writes)

    def dve(fn, reads, writes):
        return S.op("dve", fn, reads, writes)

    def pool(fn, reads, writes):
        return S.op("pool", fn, reads, writes)

    def mm(out_ap, pairs, reads, writes):
        n = len(pairs)

        def fn(e):
            ins = None
            for i, (l, r) in enumerate(pairs):
                ins = e.matmul(out_ap, l, r, start=(i == 0), stop=(i == n - 1))
            return ins
        return S.op("pe", fn, reads, writes)

    def mm_multi(groups, reads, writes):
        def fn(e):
            ins = None
            for out_ap, pairs in groups:
                n = len(pairs)
                for i, (l, r) in enumerate(pairs):
                    ins = e.matmul(out_ap, l, r, start=(i == 0), stop=(i == n - 1))
            return ins
        return S.op("pe", fn, reads, writes)

    cch = [0]

    def cload(dst_ap, src_ap, key):
        ch = "c%d" % (cch[0] % 4)
        cch[0] += 1
        S.dma("sp", ch, lambda e: e.dma_start(out=dst_ap, in_=src_ap), writes=[key])

    cload(nw[:], nw_d.ap().rearrange("p (a b) -> p a b", a=4), "nw")
    cload(wg_f[:], wg_d.ap().rearrange("p (a b) -> p a b", a=KC), "wg_f")
    cload(wf_bc[:], wf_d.ap().partition_broadcast(128), "wf_bc")
    cload(poolw_f[:], poolw_d.ap().rearrange("p (a b) -> p a b", a=4), "poolw_f")
    cload(pscale[:], pscale_d.ap(), "pscale")
    cload(gkw[:], gkw_d.ap(), "gkw")
    cload(gnw[:], gnw_d.ap(), "gnw")
    cload(cw[:], cw_d.ap().rearrange("p (a b) -> p a b", a=NCH), "cw")
    cload(cb[:], cb_d.ap(), "cb")
    cload(tri[:], tri_d.ap(), "tri")
    cload(tris[:], tris_d.ap(), "tris")
    cload(cmask[:], cmask_d.ap().rearrange("p (a b) -> p a b", a=4), "cmask")
    cload(ident_f[:], ident_d.ap(), "ident_f")
    cload(invc[:], invc_d.ap().rearrange("p (a b) -> p a b", a=4), "invc")
    dve(lambda e: e.tensor_copy(out=ident_b[:], in_=ident_f[:]), ["ident_f"], ["ident_b"])
    dve(lambda e: e.tensor_copy(out=wg_b[:], in_=wg_f[:]), ["wg_f"], ["wg_b"])
    dve(lambda e: e.tensor_copy(out=poolw_b[:], in_=poolw_f[:]), ["poolw_f"], ["poolw_b"])
    dve(lambda e: e.memset(ones_f[:], 1.0 / 128.0), [], ["ones_f"])
    dve(lambda e: e.memset(ones_b[:], 1.0), [], ["ones_b"])
    dve(lambda e: e.memset(epsc[:], EPS), [], ["epsc"])
    dve(lambda e: e.memset(gT[:], 1.0), [], ["gT"])

    for j in range(NPAN):
        S.dma("pool", "cv%d" % (j % NCV),
              lambda e, j=j: e.dma_start(out=wbf_d.ap()[j], in_=wpan_d.ap()[j]),
              writes=[("wbf", j)])

    seq_panels = []
    for s in range(nseq):
        seq_panels += ["kv%d" % i for i in range(4)]
        for t in range(ntile):
            seq_panels += TILE_PANELS
    pstate = {"next_load": 0, "next_use": 0}

    def panel_load():
        i = pstate["next_load"]
        if i >= len(seq_panels):
            return
        pstate["next_load"] = i + 1
        slot = i % RING
        pid = PID[seq_panels[i]]
        m_ = S.mute
        S.mute = False
        S.dma("sp", "ring%d" % slot,
              lambda e: e.dma_start(out=ring[slot][:].rearrange("p a b -> p (a b)"), in_=wbf_d.ap()[pid]),
              reads=[("wbf", pid)], writes=[("ring", slot)])
        S.mute = m_

    def panel_next(expect):
        i = pstate["next_use"]
        assert seq_panels[i] == expect, (seq_panels[i], expect)
        pstate["next_use"] = i + 1
        slot = i % RING
        return ring[slot], ("ring", slot)

    def initial_ring_fill():
        for _ in range(RING):
            panel_load()

    def norm_T(xap_of_block, xkeys_of_block, nblk, widx):
        ntok = nblk * 128
        for b in range(nblk):
            xin = xap_of_block(b)
            act(lambda e, b=b, xin=xin: e.activation(out=junk[:], in_=xin, func=AF.Square,
                                                     accum_out=ss[:, b:b + 1]),
                xkeys_of_block(b), ["junk", ("ss", b)])
        sskeys = [("ss", b) for b in range(nblk)]
        act(lambda e: e.activation(out=rstd[:, 0:nblk], in_=ss[:, 0:nblk], func=AF.Ln,
                                   scale=1.0 / D, bias=epsc[:, 0:1]), sskeys + ["epsc"], [("rstd", b_) for b_ in range(4)])
        act(lambda e: e.activation(out=rstd[:, 0:nblk], in_=rstd[:, 0:nblk], func=AF.Exp, scale=-0.5),
            [("rstd", b_) for b_ in range(4)], [("rstd", b_) for b_ in range(4)])
        for b in range(nblk):
            xin = xap_of_block(b)
            dve(lambda e, b=b, xin=xin: e.tensor_scalar(out=hb[:, b, :], in0=xin,
                                                        scalar1=rstd[:, b:b + 1], scalar2=None, op0=ALU.mult),
                xkeys_of_block(b) + [("rstd", b)], [("hb", b)])
        for kc in range(KC):
            pt, ptk = npt()

            def fn(e, kc=kc, pt=pt):
                ins = None
                for b in range(nblk):
                    ins = e.transpose(out=pt[:, b * 128:(b + 1) * 128],
                                      in_=hb[:, b, kc * 128:(kc + 1) * 128], identity=ident_b[:])
                return ins
            S.op("pe", fn, [("hb", b) for b in range(nblk)] + ["ident_b"], [ptk])
            if kc % 2 == 0:
                act(lambda e, kc=kc, pt=pt: e.activation(out=hT[:, kc, 0:ntok], in_=pt[:, 0:ntok], func=AF.Copy,
                                                         scale=nw[:, widx, kc:kc + 1]),
                    [ptk, "nw"], [("hT", kc)])
            else:
                dve(lambda e, kc=kc, pt=pt: e.tensor_scalar(out=hT[:, kc, 0:ntok], in0=pt[:, 0:ntok],
                                                            scalar1=nw[:, widx, kc:kc + 1], scalar2=None,
                                                            op0=ALU.mult),
                    [ptk, "nw"], [("hT", kc)])

    def norm_x(xb_, xk_, widx):
        for b in range(NB):
            xin = xb_[:, b, :]
            act(lambda e, b=b, xin=xin: e.activation(out=junk[:], in_=xin, func=AF.Square,
                                                     accum_out=ss[:, b:b + 1]), [(xk_, b)], ["junk", ("ss", b)])
            act(lambda e, b=b: e.activation(out=rstd[:, b:b + 1], in_=ss[:, b:b + 1], func=AF.Ln,
                                            scale=1.0 / D, bias=epsc[:, 0:1]), [("ss", b), "epsc"], [("rstd", b)])
            act(lambda e, b=b: e.activation(out=rstd[:, b:b + 1], in_=rstd[:, b:b + 1], func=AF.Exp, scale=-0.5),
                [("rstd", b)], [("rstd", b)])
            dve(lambda e, b=b, xin=xin: e.tensor_scalar(out=hb[:, b, :], in0=xin, scalar1=rstd[:, b:b + 1],
                                                        scalar2=None, op0=ALU.mult),
                [(xk_, b), ("rstd", b)], [("hb", b)])
            pt, ptk = npt()

            def fn(e, b=b, pt=pt):
                ins = None
                for kc in range(KC):
                    ins = e.transpose(out=pt[:, kc * 128:(kc + 1) * 128],
                                      in_=hb[:, b, kc * 128:(kc + 1) * 128], identity=ident_b[:])
                return ins
            S.op("pe", fn, [("hb", b), "ident_b"], [ptk])
            dve(lambda e, b=b, pt=pt: e.tensor_tensor(
                out=hT[:, :, b * 128:(b + 1) * 128],
                in0=pt[:, :].rearrange("p (k t) -> p k t", k=KC),
                in1=nw[:, widx, :].unsqueeze(2).broadcast_to([128, KC, 128]), op=ALU.mult),
                [ptk, "nw"], [("hT", kc) for kc in range(KC)])

    hTk = [("hT", kc) for kc in range(KC)]

    def proj_residual(src, names, xb, xk):
        for nh, nm in enumerate(names):
            pan, pk = panel_next(nm)
            for b in range(NB):
                bank, bk = nb()
                mm(bank[:, :], [(src.ap[:, kc, b * 128:(b + 1) * 128], pan[:, kc, :]) for kc in range(KC)],
                   src.k() + [pk], [bk])
                dve(lambda e, b=b, nh=nh, bank=bank: e.tensor_tensor(
                    out=xb[:, b, nh * 512:(nh + 1) * 512], in0=xb[:, b, nh * 512:(nh + 1) * 512],
                    in1=bank[:, :], op=ALU.add), [bk, (xk, b)], [(xk, b)])
            panel_load()

    def load_mem(s_):
        S.dma("sp", "mem", lambda e: e.dma_start(
            out=m_sb[:], in_=mem_d.ap()[s_ * NMEM:(s_ + 1) * NMEM, :].rearrange("(b p) d -> p b d", p=128)),
            writes=["m_sb"])

    def load_x(s_, t_):
        g = s_ * ntile + t_
        r0 = s_ * SEQ + t_ * T
        S.dma("sp", "x%d" % (g % 2), lambda e: e.dma_start(
            out=xbuf[g % 2][:], in_=x_d.ap()[r0:r0 + T, :].rearrange("(b p) d -> p b d", p=128)),
            writes=[("x%d" % (g % 2), b) for b in range(NB)])

    tile_idx = 0
    for s in range(nseq):
        if s == 0:
            load_mem(0)
            load_x(0, 0)
            initial_ring_fill()
        S.mute = en is not None and "kv" not in en
        norm_T(lambda b: m_sb[:, b, :], lambda b: ["m_sb"], 2, 2)
        for i in range(2):
            pan, pk = panel_next("kv%d" % i)
            for c in range(4):
                bank, bk = nb()
                mm(bank[:, 0:NMEM], [(pan[:, kc, c * 128:(c + 1) * 128], hT[:, kc, 0:NMEM]) for kc in range(KC)],
                   hTk + [pk], [bk])
                act(lambda e, i=i, c=c, bank=bank: e.activation(out=KT[:, i * 4 + c, :], in_=bank[:, 0:NMEM],
                                                                func=AF.Copy), [bk], [("KT", i * 4 + c)])
            panel_load()
        for i in range(2):
            pan, pk = panel_next("kv%d" % (2 + i))
            for mb in range(2):
                bank, bk = nb()
                mm(bank[:, :], [(hT[:, kc, mb * 128:(mb + 1) * 128], pan[:, kc, :]) for kc in range(KC)],
                   hTk + [pk], [bk])
                act(lambda e, i=i, mb=mb, bank=bank: e.activation(out=Vm[:, mb, i * 512:(i + 1) * 512],
                                                                  in_=bank[:, :], func=AF.Copy),
                    [bk], [("Vm", mb)])
            panel_load()
        S.mute = False
        dve(lambda e: e.memset(Sst[:], 0.0), [], ["Sst"])
        dve(lambda e: e.memset(pext[:, :, 0:16], 0.0), [], ["pext_h"])
        dve(lambda e: e.memset(halo[:], 0.0), [], ["halo"] + [("halo", q) for q in range(NCH)])

        for t in range(ntile):
            xb = xbuf[tile_idx % 2]
            xk = "x%d" % (tile_idx % 2)
            row0 = s * SEQ + t * T
            xkeys = [(xk, b) for b in range(NB)]
            if t + 1 < ntile:
                load_x(s, t + 1)
            elif s + 1 < nseq:
                load_x(s + 1, 0)
                load_mem(s + 1)

            S.mute = en is not None and "mix" not in en
            norm_x(xb, xk, 0)
            bank, bk = nb()
            mm(bank[0:16, :], [(wg_b[:, kc, :], hT[:, kc, :]) for kc in range(KC)], hTk + ["wg_b"], [bk])
            act(lambda e, bank=bank: e.activation(out=gT[0:16, :], in_=bank[0:16, :], func=AF.Copy), [bk], ["gT"])
            for b in range(NB):
                if b % 2 == 0:
                    zb, zk = nb()
                zo = zb[:, (b % 2) * 256:(b % 2 + 1) * 256]
                mm(zo, [(gT[0:17, b * 128:(b + 1) * 128], gkw[0:17, :])], ["gT", "gkw"], [zk])
                act(lambda e, b=b, zo=zo: e.activation(out=lg.ap[:, b, :], in_=zo, func=AF.Exp, scale=-1.0),
                    [zk], lg.kc(b))
                act(lambda e, b=b: e.activation(out=lg.ap[:, b, :], in_=lg.ap[:, b, :], func=AF.Ln, bias=1.0),
                    lg.kc(b), lg.kc(b))
            ck(1)
            pan, pk = panel_next("in_p")
            for c in range(4):
                bank, bk = nb()
                mm(bank[:, :], [(pan[:, kc, c * 128:(c + 1) * 128], hT[:, kc, :]) for kc in range(KC)],
                   hTk + [pk], [bk])
                act(lambda e, c=c, bank=bank: e.activation(out=pext[:, c, 16:16 + T], in_=bank[:, :], func=AF.Copy),
                    [bk], [("pext", c)])
            panel_load()
            ck(2)
            GT = []
            for pr in range(2):
                bank, bk = nb()
                mm_multi([(bank[:, b * 128:(b + 1) * 128], [(lg.ap[:, b, pr * 128:(pr + 1) * 128], tri[:, :])])
                          for b in range(NB)], lg.k() + ["tri"], [bk])
                GT.append((bank, bk))
                act(lambda e, pr=pr, bank=bank: e.activation(out=eG.ap[:, pr, :], in_=bank[:, :], func=AF.Exp),
                    [bk], eG.kc(pr))
                act(lambda e, pr=pr, bank=bank: e.activation(out=enG.ap[:, pr, :], in_=bank[:, :], func=AF.Exp,
                                                             scale=-1.0), [bk], enG.kc(pr))
                act(lambda e, pr=pr, bank=bank: e.activation(
                    out=dec[:, pr, :], in_=bank[:, :].rearrange("p (c j) -> p c j", j=64)[:, :, 63], func=AF.Exp),
                    [bk], [("dec", pr)])
            eDk = []
            for b in range(NB):
                if b % 2 == 0:
                    db, dk_ = nb()
                do = db[:, (b % 2) * 256:(b % 2 + 1) * 256]
                mm(do, [(tris[:, :], lg.ap[:, b, :])], lg.kc(b) + ["tris"], [dk_])
                act(lambda e, do=do, b=b: e.activation(out=eD.ap[:, b, :], in_=do, func=AF.Exp), [dk_], eD.kc(b))
            ck(3)
            pan, pk = panel_next("in_qk")
            for c in range(4):
                bank, bk = nb()
                mm(bank[:, :], [(pan[:, kc, c * 128:(c + 1) * 128], hT[:, kc, :]) for kc in range(KC)],
                   hTk + [pk], [bk])
                if c < 2:
                    dve(lambda e, c=c, bank=bank: e.scalar_tensor_tensor(
                        out=qdec.ap[:, c, :], in0=bank[:, :], scalar=0.125, in1=eG.ap[:, c, :],
                        op0=ALU.mult, op1=ALU.mult), [bk] + eG.kc(c), qdec.kc(c))
                else:
                    dve(lambda e, c=c, bank=bank: e.tensor_tensor(
                        out=kdec.ap[:, c - 2, :], in0=bank[:, :], in1=enG.ap[:, c - 2, :], op=ALU.mult),
                        [bk] + enG.kc(c - 2), kdec.kc(c - 2))
            for b in range(NB):
                if b % 2 == 0:
                    kb_, kk_ = nb()
                ko = kb_[:, (b % 2) * 256:(b % 2 + 1) * 256]
                mm(ko, [(hT[:, kc, b * 128:(b + 1) * 128], pan[:, kc, 256:512]) for kc in range(KC)],
                   hTk + [pk], [kk_])
                dve(lambda e, b=b, ko=ko: e.tensor_tensor(out=kte.ap[:, b, :], in0=ko, in1=eD.ap[:, b, :],
                                                          op=ALU.mult), [kk_] + eD.kc(b), kte.kc(b))
            panel_load()
            ck(4)
            pan, pk = panel_next("in_v")
            for b in range(NB):
                bank, bk = nb()
                mm(bank[:, :], [(hT[:, kc, b * 128:(b + 1) * 128], pan[:, kc, :]) for kc in range(KC)],
                   hTk + [pk], [bk])
                act(lambda e, b=b, bank=bank: e.activation(out=vtok.ap[:, b, :], in_=bank[:, :], func=AF.Copy),
                    [bk], vtok.kc(b))
            panel_load()
            ck(5)
            pextk = [("pext", c) for c in range(4)]
            for g in range(4):
                src_ap = pext[:, g, :]
                src_k = [("pext", g), "pext_h"]
                L = 16 + T
                sh = 1
                for it in range(g + 1):
                    dst = pa[it % 2]
                    dve(lambda e, src_ap=src_ap, dst=dst, sh=sh, L=L: e.tensor_tensor(
                        out=dst.ap[:, 2 * sh - 1:L], in0=src_ap[:, 2 * sh - 1:L], in1=src_ap[:, sh - 1:L - sh],
                        op=ALU.add),
                        src_k, dst.k())
                    src_ap = dst.ap
                    src_k = dst.k()
                    sh *= 2
                w = 2 ** (g + 1)
                dve(lambda e, g=g, src_ap=src_ap, w=w: e.scalar_tensor_tensor(
                    out=pooled.ap[:, g, :], in0=src_ap[:, 16:16 + T], scalar=1.0 / w, in1=pext[:, g, 16:16 + T],
                    op0=ALU.mult, op1=ALU.subtract), src_k + [("pext", g)], pooled.kc(g))
                if t == 0:
                    dve(lambda e, g=g, src_ap=src_ap: e.tensor_tensor(
                        out=src_ap[:, 16:32], in0=src_ap[:, 16:32], in1=invc[:, g, :], op=ALU.mult),
                        src_k + ["invc"], src_k)
                    dve(lambda e, g=g, src_ap=src_ap: e.tensor_tensor(
                        out=pooled.ap[:, g, 0:16], in0=src_ap[:, 16:32], in1=pext[:, g, 16:32], op=ALU.subtract),
                        src_k + [("pext", g)], pooled.kc(g))
                bank, bk = nb()
                mm(bank[:, :], [(poolw_b[:, g, :], pooled.ap[:, g, :])], pooled.kc(g) + ["poolw_b"], [bk])
                act(lambda e, g=g, bank=bank: e.activation(out=catT.ap[:, g, :], in_=bank[:, :], func=AF.Copy,
                                                           scale=pscale[:, g:g + 1]), [bk, "pscale"], catT.kc(g))
            dve(lambda e: e.tensor_copy(out=pext[:, :, 0:16], in_=pext[:, :, T:T + 16]), pextk + ["pext_h"],
                ["pext_h"])
            ck(7)
            for hh in range(2):
                for hl in range(2):
                    scb, sck = nb()
                    groups = []
                    for bl in range(2):
                        b = 2 * hh + bl
                        for hf in range(2):
                            c = 2 * b + hf
                            for hp in range(2):
                                groups.append((scb[64 * hf:64 * hf + 64, bl * 128 + hp * 64: bl * 128 + hp * 64 + 64],
                                               [(kdec.ap[64 * hl:64 * hl + 64, hp, c * 64:(c + 1) * 64],
                                                 qdec.ap[64 * hl:64 * hl + 64, hp, c * 64:(c + 1) * 64])]))
                    mm_multi(groups, kdec.k() + qdec.k(), [sck])
                    dve(lambda e, hh=hh, hl=hl, scb=scb: e.tensor_tensor(
                        out=scm.ap[:, 2 * hh:2 * hh + 2, hl * 128:(hl + 1) * 128],
                        in0=scb[:, 0:256].rearrange("p (a b) -> p a b", a=2),
                        in1=cmask2[:, :, :],
                        op=ALU.mult), [sck, "cmask"], scm.kc(2 * hh, 2 * hh + 2))
                ck(8)
                kvb = []
                for hf in range(2):
                    bank, bk = nb()
                    groups = []
                    for bl in range(2):
                        b = 2 * hh + bl
                        for h in range(4):
                            pr, hl = divmod(h, 2)
                            groups.append((bank[64 * hl:64 * hl + 64, bl * 256 + pr * 128: bl * 256 + pr * 128 + 128],
                                           [(kte.ap[64 * hf:64 * hf + 64, b, h * 64:(h + 1) * 64],
                                             vtok.ap[64 * hf:64 * hf + 64, b, h * 128:(h + 1) * 128])]))
                    mm_multi(groups, kte.kc(2 * hh, 2 * hh + 2) + vtok.kc(2 * hh, 2 * hh + 2), [bk])
                    kvb.append((bank, bk))
                ck(9)
                for cl in range(4):
                    c = 4 * hh + cl
                    bl, hf = divmod(cl, 2)
                    bank, bk = kvb[hf]
                    act(lambda e, c=c: e.activation(out=Sbf.ap[:, c, :], in_=Sst[:].rearrange("p a b -> p (a b)"),
                                                    func=AF.Copy), ["Sst"], Sbf.kc(c))
                    for pr in range(2):
                        dve(lambda e, c=c, pr=pr, bank=bank, bl=bl: e.scalar_tensor_tensor(
                            out=Sst[:, pr, :], in0=Sst[:, pr, :], scalar=dec[:, pr, c:c + 1],
                            in1=bank[:, bl * 256 + pr * 128: bl * 256 + pr * 128 + 128],
                            op0=ALU.mult, op1=ALU.add), ["Sst", bk, ("dec", pr)], ["Sst"])
            ck(6)
            pan, pk = panel_next("in_r")
            for c in range(4):
                bank, bk = nb()
                mm(bank[:, :], [(pan[:, kc, c * 128:(c + 1) * 128], hT[:, kc, :]) for kc in range(KC)],
                   hTk + [pk], [bk])
                act(lambda e, c=c, bank=bank: e.activation(out=sr.ap[:, c, :], in_=bank[:, :], func=AF.Silu),
                    [bk], sr.kc(c))
            panel_load()
            for hh in range(2):
                ck(10)
                oib = []
                for hf in range(2):
                    bank, bk = nb()
                    groups = []
                    for bl in range(2):
                        b = 2 * hh + bl
                        for h in range(4):
                            hp, hl = divmod(h, 2)
                            groups.append((bank[:, bl * 256 + h * 64: bl * 256 + h * 64 + 64],
                                           [(vtok.ap[64 * hf:64 * hf + 64, b, h * 128:(h + 1) * 128],
                                             scm.ap[64 * hf:64 * hf + 64, b, (hl * 2 + hp) * 64:(hl * 2 + hp) * 64 + 64])]))
                    mm_multi(groups, vtok.kc(2 * hh, 2 * hh + 2) + scm.kc(2 * hh, 2 * hh + 2), [bk])
                    oib.append((bank, bk))
                ck(11)
                for hl in range(2):
                    bank, bk = nb()
                    groups = []
                    for cl in range(4):
                        c = 4 * hh + cl
                        for hp in range(2):
                            groups.append((bank[:, cl * 128 + hp * 64: cl * 128 + hp * 64 + 64],
                                           [(Sbf.ap[64 * hl:64 * hl + 64, c, hp * 128:(hp + 1) * 128],
                                             qdec.ap[64 * hl:64 * hl + 64, hp, c * 64:(c + 1) * 64])]))
                    mm_multi(groups, Sbf.kc(4 * hh, 4 * hh + 4) + qdec.k(), [bk])
                    for hp in range(2):
                        act(lambda e, hl=hl, hp=hp, hh=hh, bank=bank: e.activation(
                            out=o_sb.ap[:, 2 * hp + hl, hh * 256:(hh + 1) * 256].rearrange("p (c i) -> p c i", i=64),
                            in_=bank[:, :].rearrange("p (c q i) -> p c q i", q=2, i=64)[:, :, hp, :],
                            func=AF.Copy), [bk], o_sb.kc(2 * hp + hl))
                ck(12)
                for hf in range(2):
                    bank, bk = oib[hf]
                    for bl in range(2):
                        cl = 2 * bl + hf
                        dve(lambda e, hh=hh, bl=bl, cl=cl, bank=bank: e.tensor_tensor(
                            out=o_sb.ap[:, :, hh * 256 + cl * 64: hh * 256 + cl * 64 + 64],
                            in0=o_sb.ap[:, :, hh * 256 + cl * 64: hh * 256 + cl * 64 + 64],
                            in1=bank[:, bl * 256:(bl + 1) * 256].rearrange("p (h i) -> p h i", i=64),
                            op=ALU.add), [bk] + o_sb.k(), o_sb.k())
            for h in range(4):
                act(lambda e, h=h: e.activation(out=sq.ap[:, h, :], in_=o_sb.ap[:, h, :], func=AF.Square),
                    o_sb.kc(h), sq.kc(h))
            ck(13)
            for h in range(4):
                bank, bk = nb()
                mm(bank[:, :], [(ones_f[:, :], sq.ap[:, h, :])], sq.kc(h) + ["ones_f"], [bk])
                r_ = rb[h % 2]
                act(lambda e, bank=bank, r_=r_: e.activation(out=r_.ap, in_=bank[:, :], func=AF.Ln,
                                                             bias=epsc[:, 0:1]), [bk, "epsc"], r_.k())
                act(lambda e, r_=r_: e.activation(out=r_.ap, in_=r_.ap, func=AF.Exp, scale=-0.5), r_.k(), r_.k())
                dve(lambda e, h=h, r_=r_: e.scalar_tensor_tensor(
                    out=gl.ap, in0=o_sb.ap[:, h, :], scalar=gnw[:, 0:1], in1=r_.ap, op0=ALU.mult, op1=ALU.mult),
                    o_sb.kc(h) + r_.k() + ["gnw"], gl.k())
                dve(lambda e, h=h: e.tensor_tensor(out=catT.ap[:, 4 + h, :], in0=gl.ap, in1=sr.ap[:, h, :],
                                                   op=ALU.mult), gl.k() + sr.kc(h), catT.kc(4 + h))
            ck(14)
            proj_residual(catT, ["wout0", "wout1"], xb, xk)

            S.mute = en is not None and "xat" not in en
            norm_x(xb, xk, 1)
            for i in range(2):
                pan, pk = panel_next("wq%d" % i)
                for c in range(4):
                    bank, bk = nb()
                    mm(bank[:, :], [(pan[:, kc, c * 128:(c + 1) * 128], hT[:, kc, :]) for kc in range(KC)],
                       hTk + [pk], [bk])
                    cc = i * 4 + c
                    if c % 2 == 0:
                        act(lambda e, cc=cc, bank=bank: e.activation(out=qxT.ap[:, cc, :], in_=bank[:, :],
                                                                     func=AF.Copy), [bk], qxT.kc(cc))
                    else:
                        dve(lambda e, cc=cc, bank=bank: e.tensor_copy(out=qxT.ap[:, cc, :], in_=bank[:, :]),
                            [bk], qxT.kc(cc))
                panel_load()
            for h in range(4):
                for mc in range(2):
                    bank, bk = nb()
                    mm(bank[:, :], [(KT[:, 2 * h + dc, mc * 128:(mc + 1) * 128], qxT.ap[:, 2 * h + dc, :])
                                    for dc in range(2)],
                       qxT.kc(2 * h, 2 * h + 2) + [("KT", 2 * h), ("KT", 2 * h + 1)], [bk])
                    act(lambda e, h=h, mc=mc, bank=bank: e.activation(out=PT.ap[:, 2 * h + mc, :], in_=bank[:, :],
                                                                      func=AF.Exp, scale=1.0 / 16.0),
                        [bk], PT.kc(2 * h + mc))
                smb, smk = nb()
                mm(smb[:, :], [(ones_b[:, :], PT.ap[:, 2 * h + mc, :]) for mc in range(2)],
                   PT.kc(2 * h, 2 * h + 2) + ["ones_b"], [smk])
                r_ = rs[h % 2]
                dve(lambda e, smb=smb, r_=r_: e.reciprocal(out=r_.ap, in_=smb[:, :]), [smk], r_.k())
                for dc in range(2):
                    bank, bk = nb()
                    mm(bank[:, :], [(Vm[:, mc, h * 256 + dc * 128: h * 256 + dc * 128 + 128], PT.ap[:, 2 * h + mc, :])
                                    for mc in range(2)],
                       PT.kc(2 * h, 2 * h + 2) + [("Vm", 0), ("Vm", 1)], [bk])
                    dve(lambda e, h=h, dc=dc, bank=bank, r_=r_: e.tensor_tensor(
                        out=oxT.ap[:, 2 * h + dc, :], in0=bank[:, :], in1=r_.ap, op=ALU.mult),
                        [bk] + r_.k(), oxT.kc(2 * h + dc))
            proj_residual(oxT, ["wo0", "wo1"], xb, xk)

            S.mute = en is not None and "ffn" not in en
            norm_x(xb, xk, 3)
            for j in range(11):
                pan, pk = panel_next("up%d" % j)
                ys = []
                for r4 in range(4):
                    q = 4 * j + r4
                    bank, bk = nb()
                    mm(bank[:, :], [(pan[:, kc, r4 * 128:(r4 + 1) * 128], hT[:, kc, :]) for kc in range(KC)],
                       hTk + [pk], [bk])
                    ub = ubuf[q % 4]
                    yb = ybuf[q % 4]
                    act(lambda e, q=q, ub=ub: e.activation(out=ub.ap[:, 0:2], in_=halo[:, q, :], func=AF.Copy),
                        [("halo", q), "halo"], ub.k())
                    act(lambda e, ub=ub, bank=bank: e.activation(out=ub.ap[:, 2:2 + T], in_=bank[:, :], func=AF.Copy),
                        [bk], ub.k())
                    act(lambda e, q=q, yb=yb, bank=bank: e.activation(
                        out=yb.ap, in_=bank[:, :], func=AF.Identity, scale=cw[:, q, 2:3], bias=cb[:, q:q + 1]),
                        [bk, "cw", "cb"], yb.k())
                    act(lambda e, q=q, bank=bank: e.activation(out=halo[:, q, :], in_=bank[:, T - 2:T], func=AF.Copy),
                        [bk], [("halo", q)])
                    dve(lambda e, q=q, ub=ub, yb=yb: e.scalar_tensor_tensor(
                        out=yb.ap, in0=ub.ap[:, 1:1 + T], scalar=cw[:, q, 1:2], in1=yb.ap,
                        op0=ALU.mult, op1=ALU.add), ub.k() + yb.k() + ["cw"], yb.k())
                    dve(lambda e, q=q, ub=ub, yb=yb: e.scalar_tensor_tensor(
                        out=yb.ap, in0=ub.ap[:, 0:T], scalar=cw[:, q, 0:1], in1=yb.ap,
                        op0=ALU.mult, op1=ALU.add), ub.k() + yb.k() + ["cw"], yb.k())
                    ys.append(yb)
                panel_load()
                for r2 in range(2):
                    ch = 2 * j + r2
                    sgb = sg[ch % 2]
                    yg, yv = ys[r2], ys[2 + r2]
                    act(lambda e, yg=yg, sgb=sgb: e.activation(out=sgb.ap, in_=yg.ap, func=AF.Silu),
                        yg.k(), sgb.k())
                    dve(lambda e, ch=ch, sgb=sgb, yv=yv: e.tensor_tensor(out=actT.ap[:, ch, :], in0=sgb.ap,
                                                                         in1=yv.ap, op=ALU.mult),
                        sgb.k() + yv.k(), actT.kc(ch))
            for nh in range(2):
                banks = [nb() for _ in range(NB)]
                for kg in range(3):
                    pan, pk = panel_next("dn%d_%d" % (nh, kg))
                    nk = 8 if kg < 2 else 6
                    for b in range(NB):
                        bank, bk = banks[b]

                        def fn(e, b=b, bank=bank, kg=kg, nk=nk, pan=pan):
                            ins = None
                            for kc in range(nk):
                                ins = e.matmul(bank[:, :], actT.ap[:, kg * 8 + kc, b * 128:(b + 1) * 128],
                                               pan[:, kc, :], start=(kg == 0 and kc == 0),
                                               stop=(kg == 2 and kc == nk - 1))
                            return ins
                        S.op("pe", fn, actT.kc(kg * 8, kg * 8 + nk) + [pk], [bk])
                    panel_load()
                for b in range(NB):
                    bank, bk = banks[b]
                    dve(lambda e, b=b, nh=nh, bank=bank, xb=xb: e.tensor_tensor(
                        out=xb[:, b, nh * 512:(nh + 1) * 512], in0=xb[:, b, nh * 512:(nh + 1) * 512],
                        in1=bank[:, :], op=ALU.add), [bk, (xk, b)], [(xk, b)])

            S.mute = en is not None and "fin" not in en
            for b in range(NB):
                act(lambda e, b=b, xb=xb: e.activation(out=junk[:], in_=xb[:, b, :], func=AF.Square,
                                                accum_out=ss[:, b:b + 1]), [(xk, b)], ["junk", ("ss", b)])
            act(lambda e: e.activation(out=rstd[:, :], in_=ss[:, :], func=AF.Ln, scale=1.0 / D, bias=epsc[:, 0:1]),
                [("ss", b) for b in range(NB)] + ["epsc"], [("rstd", b_) for b_ in range(4)])
            act(lambda e: e.activation(out=rstd[:, :], in_=rstd[:, :], func=AF.Exp, scale=-0.5),
                [("rstd", b_) for b_ in range(4)], [("rstd", b_) for b_ in range(4)])
            for b in range(NB):
                dve(lambda e, b=b, xb=xb: e.scalar_tensor_tensor(out=xb[:, b, :], in0=xb[:, b, :], scalar=rstd[:, b:b + 1],
                                                          in1=wf_bc[:, :], op0=ALU.mult, op1=ALU.mult),
                    [(xk, b), ("rstd", b), "wf_bc"], [(xk, b)])
            S.mute = False
            S.dma("sp", xk, lambda e, xb=xb, row0=row0: e.dma_start(
                out=out_d.ap()[row0:row0 + T, :].rearrange("(b p) d -> p b d", p=128), in_=xb[:]),
                reads=xkeys, writes=[("out", tile_idx)])
            tile_idx += 1

    S.wait_all("sp", [S.lastw[("out", i)] for i in range(tile_idx)])
    S.emit()
    return nc


def _prep_shared(inp):
    f = lambda a: np.ascontiguousarray(np.asarray(a, dtype=np.float32))
    w_in = f(inp["w_in"])[0]
    tri, tris, cmask, ident, invc = _consts()
    col = lambda v, n: np.ascontiguousarray(f(v).reshape(n, 128).T)
    nw = np.stack([col(inp["norm_mix_w"][0], 8), col(inp["norm_xattn_w"][0], 8),
                   col(inp["norm_mem_w"][0], 8), col(inp["norm_ffn_w"][0], 8)], axis=1)
    wg = w_in[:, 1536:1552].reshape(8, 128, 16).transpose(1, 0, 2)
    poolw = f(inp["pool_w"])[0].transpose(1, 0, 2)
    gkw = np.concatenate([f(inp["gk_w2"])[0], f(inp["gk_b"])[0][None, :]], axis=0)
    qcols = np.array([_chunk_col(q) for q in range(NCH)])
    idx = qcols[:, None] + np.arange(128)[None, :]
    cwf = f(inp["ffn_conv_w"])[0]
    cw = cwf[:, idx].transpose(2, 1, 0)
    cb = f(inp["ffn_conv_b"])[0][idx].T
    shared = {
        "wpan": _pack_weights(w_in, f(inp["w_out"])[0], f(inp["xattn_wq"])[0], f(inp["xattn_wkv"])[0],
                              f(inp["xattn_wo"])[0], f(inp["ffn_w_up"])[0], f(inp["ffn_w_down"])[0]),
        "wg": np.ascontiguousarray(wg).reshape(128, 128),
        "nw": np.ascontiguousarray(nw).reshape(128, 32),
        "wf": f(inp["norm_final_w"]).reshape(1, D),
        "poolw": np.ascontiguousarray(poolw).reshape(128, 512),
        "pscale": col(inp["pool_scale"][0], 4),
        "gkw": np.ascontiguousarray(gkw),
        "gnw": f(inp["gla_norm_w"])[0].reshape(128, 1),
        "cw": np.ascontiguousarray(cw).reshape(128, NCH * 3),
        "cb": np.ascontiguousarray(cb),
        "tri": tri, "tris": tris, "cmask": cmask, "ident": ident, "invc": invc,
    }
    return shared


def kernel(**inputs):
    x = np.asarray(inputs["x"], dtype=np.float32)
    mem = np.asarray(inputs["mem"], dtype=np.float32)
    shared = _prep_shared(inputs)
    nc = build_program()
    in_maps = []
    for c in range(NCORES):
        m = dict(shared)
        m["x"] = np.ascontiguousarray(x[c * SEQ_PER_CORE:(c + 1) * SEQ_PER_CORE]).reshape(SEQ_PER_CORE * SEQ, D)
        m["mem"] = np.ascontiguousarray(mem[c * SEQ_PER_CORE:(c + 1) * SEQ_PER_CORE]).reshape(SEQ_PER_CORE * NMEM, D)
        in_maps.append(m)
    res = run_bass_kernel_spmd(nc, in_maps, core_ids=list(range(NCORES)))
    out = np.concatenate([r["out"].reshape(SEQ_PER_CORE, SEQ, D) for r in res.results], axis=0)
    return out.astype(np.float32)
```

```python
import contextlib
import numpy as np
import concourse.bass as bass
import concourse.mybir as mybir
from concourse.bass_utils import run_bass_kernel_spmd

F32 = mybir.dt.float32
BF16 = mybir.dt.bfloat16
AF = mybir.ActivationFunctionType
ALU = mybir.AluOpType

D = 1024
SEQ = 2048
NMEM = 256
T = 512
NB = 4
KC = 8
DFF = 2816
NCH = 44
EPS = 1e-6
NCORES = 8
SEQ_PER_CORE = 4
RING = 4
NCV = 2
STRICT = True


class Sched:
    ENGS = ("pe", "act", "dve", "pool", "sp")

    def __init__(self, nc):
        self.nc = nc
        self.ops = {e: [] for e in self.ENGS}
        self.cnt = {}
        self.seen = {e: {} for e in self.ENGS}
        self.lastw = {}
        self.readers = {}
        self.chan_last = {}
        self.mute = False

    def _deps(self, reads, writes, eng=None):
        deps = []
        own = ("c", eng)
        for b in reads:
            t = self.lastw.get(b)
            if t is not None:
                deps.append(t)
            if isinstance(b, tuple) and b[0] in ("ps", "pt"):
                r = self.readers.get(b)
                if r:
                    deps.extend((k, v) for k, v in r.items() if k != own)
        for b in writes:
            t = self.lastw.get(b)
            if t is not None and (STRICT or t[0] != own):
                deps.append(t)
            r = self.readers.get(b)
            if r:
                deps.extend((k, v) for k, v in r.items() if (STRICT or k != own))
        return deps

    def _commit(self, tok, reads, writes):
        k, v = tok
        for b in reads:
            d = self.readers.setdefault(b, {})
            if d.get(k, 0) < v:
                d[k] = v
        for b in writes:
            self.lastw[b] = tok
            self.readers[b] = {}

    def _waits(self, eng, deps):
        need = {}
        for (k, v) in deps:
            if eng == "pe" and k == ("c", "pe"):
                continue
            if need.get(k, 0) < v:
                need[k] = v
        out = []
        seen = self.seen[eng]
        for k, v in need.items():
            if seen.get(k, 0) < v:
                seen[k] = v
                out.append((k, v))
        return out

    def op(self, eng, fn, reads=(), writes=()):
        if self.mute:
            return None
        waits = self._waits(eng, self._deps(reads, writes, eng))
        k = ("c", eng)
        self.cnt[k] = self.cnt.get(k, 0) + 1
        tok = (k, self.cnt[k])
        self._commit(tok, reads, writes)
        self.ops[eng].append((waits, fn, k, 1))
        return tok

    def dma(self, eng, chan, fn, reads=(), writes=()):
        if self.mute:
            return None
        deps = self._deps(reads, writes)
        k = ("d", chan)
        if k in self.chan_last:
            deps.append(self.chan_last[k])
        waits = self._waits(eng, deps)
        self.cnt[k] = self.cnt.get(k, 0) + 16
        tok = (k, self.cnt[k])
        self.chan_last[k] = tok
        self._commit(tok, reads, writes)
        self.ops[eng].append((waits, fn, k, 16))
        return tok

    def wait_all(self, eng, toks):
        waits = self._waits(eng, [t for t in toks if t is not None])
        self.ops[eng].append((waits, None, None, 0))

    def emit(self):
        nc = self.nc
        sems = {}
        with contextlib.ExitStack() as st:
            for k in self.cnt:
                sems[k] = st.enter_context(nc.semaphore("s_%s_%s" % k))
            block = st.enter_context(nc.Block())

            def run(engobj, e):
                for (waits, fn, semk, incv) in self.ops[e]:
                    for (k, v) in waits:
                        engobj.wait_ge(sems[k], v)
                    if fn is not None:
                        ins = fn(engobj)
                        ins.then_inc(sems[semk], incv)

            @block.tensor
            def _(eng):
                run(eng, "pe")

            @block.scalar
            def _(eng):
                run(eng, "act")

            @block.vector
            def _(eng):
                run(eng, "dve")

            @block.gpsimd
            def _(eng):
                run(eng, "pool")

            @block.sync
            def _(eng):
                run(eng, "sp")


def _chunk_col(q):
    j, r = divmod(q, 4)
    return (2 * j + (r % 2)) * 128 + (DFF if r >= 2 else 0)


def _panel(W, r0, nk, cols):
    P = np.zeros((128, KC, 512), np.float32)
    blk = W[r0:r0 + nk * 128][:, cols]
    P[:, :nk, :] = blk.reshape(nk, 128, 512).transpose(1, 0, 2)
    return P.reshape(128, KC * 512)


PANEL_NAMES = (["kv%d" % i for i in range(4)] + ["in_p", "in_qk", "in_v", "in_r", "wout0", "wout1",
               "wq0", "wq1", "wo0", "wo1"] + ["up%d" % i for i in range(11)] +
               ["dn%d_%d" % (nh, kg) for nh in range(2) for kg in range(3)])
PID = {n: i for i, n in enumerate(PANEL_NAMES)}
NPAN = len(PANEL_NAMES)
TILE_PANELS = (["in_p", "in_qk", "in_v", "in_r", "wout0", "wout1", "wq0", "wq1", "wo0", "wo1"] +
               ["up%d" % i for i in range(11)] + ["dn%d_%d" % (nh, kg) for nh in range(2) for kg in range(3)])


def _pack_weights(w_in, w_out, wq, wkv, wo, w_up, w_down):
    ar = np.arange(512)
    out = np.zeros((NPAN, 128, KC * 512), np.float32)
    for i in range(4):
        out[PID["kv%d" % i]] = _panel(wkv, 0, 8, ar + 512 * i)
    out[PID["in_p"]] = _panel(w_in, 0, 8, ar)
    out[PID["in_qk"]] = _panel(w_in, 0, 8, ar + 512)
    out[PID["in_v"]] = _panel(w_in, 0, 8, ar + 1024)
    out[PID["in_r"]] = _panel(w_in, 0, 8, ar + 1552)
    for i in range(2):
        out[PID["wout%d" % i]] = _panel(w_out, 0, 8, ar + 512 * i)
        out[PID["wq%d" % i]] = _panel(wq, 0, 8, ar + 512 * i)
        out[PID["wo%d" % i]] = _panel(wo, 0, 8, ar + 512 * i)
    for j in range(11):
        cols = np.concatenate([np.arange(128) + _chunk_col(4 * j + r) for r in range(4)])
        out[PID["up%d" % j]] = _panel(w_up, 0, 8, cols)
    for nh in range(2):
        for kg in range(3):
            nk = 8 if kg < 2 else 6
            out[PID["dn%d_%d" % (nh, kg)]] = _panel(w_down, kg * 1024, nk, ar + 512 * nh)
    return out


def _consts():
    s = np.arange(128)[:, None]
    t = np.arange(128)[None, :]
    same = (s // 64) == (t // 64)
    tri = np.where(same & (s <= t), -1.0 / 16.0, 0.0).astype(np.float32)
    tris = np.where(same & (s > t), -1.0 / 16.0, 0.0).astype(np.float32)
    j = (np.arange(128) % 64)[:, None, None]
    i = np.arange(64)[None, None, :]
    cmask = np.broadcast_to((j <= i), (128, 4, 64)).astype(np.float32).reshape(128, 256)
    ident = np.eye(128, dtype=np.float32)
    invc = np.zeros((128, 4, 16), np.float32)
    tt = np.arange(16)
    for g, w in enumerate((2, 4, 8, 16)):
        invc[:, g, :] = 1.0 / np.minimum(tt + 1, w)
    return tri, tris, cmask, ident, invc.reshape(128, 64)


def build_program(nseq=SEQ_PER_CORE, ntile=SEQ // T, dbg=None):
    nc = bass.Bass("TRN2", target_bir_lowering=False)
    S = Sched(nc)
    en = dbg
    import os as _os
    MIXSTOP = int(_os.environ.get("MIXSTOP", "99"))

    def ck(n):
        if n > MIXSTOP:
            S.mute = True

    def din(name, shape):
        return nc.dram_tensor(name, list(shape), F32, kind="ExternalInput")

    x_d = din("x", [nseq * SEQ, D])
    mem_d = din("mem", [nseq * NMEM, D])
    wpan_d = din("wpan", [NPAN, 128, KC * 512])
    wg_d = din("wg", [128, KC * 16])
    nw_d = din("nw", [128, 4 * KC])
    wf_d = din("wf", [1, D])
    poolw_d = din("poolw", [128, 4 * 128])
    pscale_d = din("pscale", [128, 4])
    gkw_d = din("gkw", [17, 256])
    gnw_d = din("gnw", [128, 1])
    cw_d = din("cw", [128, NCH * 3])
    cb_d = din("cb", [128, NCH])
    tri_d = din("tri", [128, 128])
    tris_d = din("tris", [128, 128])
    cmask_d = din("cmask", [128, 256])
    ident_d = din("ident", [128, 128])
    invc_d = din("invc", [128, 64])
    out_d = nc.dram_tensor("out", [nseq * SEQ, D], F32, kind="ExternalOutput")
    wbf_d = nc.dram_tensor("wbf", [NPAN, 128, KC * 512], BF16, kind="Internal")

    def sb(name, shape, dt=F32):
        return nc.alloc_sbuf_tensor("s_" + name, list(shape), dt)

    xbuf = [sb("xb%d" % i, [128, NB, D]) for i in range(2)]
    ring = [sb("ring%d" % i, [128, KC, 512], BF16) for i in range(RING)]
    hT = sb("hT", [128, KC, T], BF16)
    hb = sb("hb", [128, NB, D], BF16)
    junk = sb("junk", [128, D], BF16)
    ss = sb("ss", [128, 4])
    rstd = sb("rstd", [128, 4])
    epsc = sb("epsc", [128, 1])
    wf_bc = sb("wf_bc", [128, D])
    nw = sb("nw", [128, 4, KC])
    wg_f = sb("wg_f", [128, KC, 16])
    wg_b = sb("wg_b", [128, KC, 16], BF16)
    poolw_f = sb("poolw_f", [128, 4, 128])
    poolw_b = sb("poolw_b", [128, 4, 128], BF16)
    pscale = sb("pscale", [128, 4])
    gkw = sb("gkw", [17, 256])
    gnw = sb("gnw", [128, 1])
    cw = sb("cw", [128, NCH, 3])
    cb = sb("cb", [128, NCH])
    tri = sb("tri", [128, 128])
    tris = sb("tris", [128, 128])
    cmask = sb("cmask", [128, 4, 64])
    cmask2 = cmask[:].rearrange("p (a b) c -> p a (b c)", a=2)
    ident_f = sb("ident_f", [128, 128])
    ident_b = sb("ident_b", [128, 128], BF16)
    invc = sb("invc", [128, 4, 16])
    ones_f = sb("ones_f", [128, 128])
    ones_b = sb("ones_b", [128, 128], BF16)
    KT = sb("KT", [128, 8, NMEM], BF16)
    Vm = sb("Vm", [128, 2, D], BF16)
    pext = sb("pext", [128, 4, 16 + T])
    Sst = sb("Sst", [128, 2, 128])
    halo = sb("halo", [128, NCH, 2])
    dec = sb("dec", [128, 2, 8])
    gT = sb("gT", [17, T])
    m_sb = sb("m_sb", [128, 2, D])

    ARENA_KB = 80
    arena = sb("arena", [128, ARENA_KB * 256])

    class AV:
        def __init__(self, off_kb, shape, dt):
            esz = 4 if dt == F32 else 2
            n = int(np.prod(shape))
            self.off = off_kb * 1024
            self.esz = esz
            self.n = n
            assert self.off + n * esz <= ARENA_KB * 1024, (off_kb, shape)
            a = arena[:, self.off // 4:(self.off + n * esz + 3) // 4]
            if dt != F32:
                a = a.bitcast(dt)
            if len(shape) == 2:
                a = a.rearrange("p (a b) -> p a b", a=shape[0])
            elif len(shape) == 3:
                a = a.rearrange("p (a b c) -> p a b c", a=shape[0], b=shape[1])
            self.ap = a
            self.shape = shape

        def k(self, lo=0, hi=None):
            hi = self.n if hi is None else hi
            b0 = (self.off + lo * self.esz) // 1024
            b1 = (self.off + hi * self.esz - 1) // 1024
            return [("ar", i) for i in range(b0, b1 + 1)]

        def kc(self, i, j=None):
            per = self.n // self.shape[0]
            j = i + 1 if j is None else j
            return self.k(i * per, j * per)

    catT = AV(0, [8, T], BF16)
    o_sb = AV(8, [4, T], F32)
    sq = AV(16, [4, T], F32)
    rb = [AV(24 + 2 * i, [T], F32) for i in range(2)]
    gl = AV(28, [T], F32)
    lg = AV(30, [4, 256], F32)
    eD = AV(34, [4, 256], F32)
    eG = AV(38, [2, T], F32)
    enG = AV(42, [2, T], F32)
    qdec = AV(46, [2, T], BF16)
    kdec = AV(48, [2, T], BF16)
    kte = AV(50, [4, 256], BF16)
    vtok = AV(52, [4, T], BF16)
    scm = AV(56, [4, 256], BF16)
    sr = AV(58, [4, T], F32)
    pooled = AV(66, [4, T], BF16)
    pa = [AV(70 + 3 * i, [16 + T], F32) for i in range(2)]
    Sbf = AV(76, [8, 256], BF16)
    oxT = AV(0, [8, T], BF16)
    qxT = AV(8, [8, T], BF16)
    PT = AV(16, [8, T], BF16)
    rs = [AV(24 + 2 * i, [T], F32) for i in range(2)]
    actT = AV(8, [22, T], BF16)
    ubuf = [AV(30 + 3 * i, [2 + T], F32) for i in range(4)]
    ybuf = [AV(42 + 2 * i, [T], F32) for i in range(4)]
    sg = [AV(50 + 2 * i, [T], F32) for i in range(2)]

    NPS = 6
    psb = [nc.alloc_psum_tensor("ps%d" % i, [128, 512], F32) for i in range(NPS)]
    ptb = [nc.alloc_psum_tensor("pt%d" % i, [128, 1024], BF16) for i in range(2)]
    rr = {"ps": 0, "pt": 0}

    def nb():
        i = rr["ps"]
        rr["ps"] = (i + 1) % NPS
        return psb[i], ("ps", i)

    def npt():
        i = rr["pt"]
        rr["pt"] = (i + 1) % 2
        return ptb[i], ("pt", i)

    def act(fn, reads, writes):
        return S.op("act", fn, reads, writes)

    def dve(fn, reads, writes):
        return S.op("dve", fn, reads, writes)

    def pool(fn, reads, writes):
        return S.op("pool", fn, reads, writes)

    def mm(out_ap, pairs, reads, writes):
        n = len(pairs)

        def fn(e):
            ins = None
            for i, (l, r) in enumerate(pairs):
                ins = e.matmul(out_ap, l, r, start=(i == 0), stop=(i == n - 1))
            return ins
        return S.op("pe", fn, reads, writes)

    def mm_multi(groups, reads, writes):
        def fn(e):
            ins = None
            for out_ap, pairs in groups:
                n = len(pairs)
                for i, (l, r) in enumerate(pairs):
                    ins = e.matmul(out_ap, l, r, start=(i == 0), stop=(i == n - 1))
            return ins
        return S.op("pe", fn, reads, writes)

    cch = [0]

    def cload(dst_ap, src_ap, key):
        ch = "c%d" % (cch[0] % 4)
        cch[0] += 1
        S.dma("sp", ch, lambda e: e.dma_start(out=dst_ap, in_=src_ap), writes=[key])

    cload(nw[:], nw_d.ap().rearrange("p (a b) -> p a b", a=4), "nw")
    cload(wg_f[:], wg_d.ap().rearrange("p (a b) -> p a b", a=KC), "wg_f")
    cload(wf_bc[:], wf_d.ap().partition_broadcast(128), "wf_bc")
    cload(poolw_f[:], poolw_d.ap().rearrange("p (a b) -> p a b", a=4), "poolw_f")
    cload(pscale[:], pscale_d.ap(), "pscale")
    cload(gkw[:], gkw_d.ap(), "gkw")
    cload(gnw[:], gnw_d.ap(), "gnw")
    cload(cw[:], cw_d.ap().rearrange("p (a b) -> p a b", a=NCH), "cw")
    cload(cb[:], cb_d.ap(), "cb")
    cload(tri[:], tri_d.ap(), "tri")
    cload(tris[:], tris_d.ap(), "tris")
    cload(cmask[:], cmask_d.ap().rearrange("p (a b) -> p a b", a=4), "cmask")
    cload(ident_f[:], ident_d.ap(), "ident_f")
    cload(invc[:], invc_d.ap().rearrange("p (a b) -> p a b", a=4), "invc")
    dve(lambda e: e.tensor_copy(out=ident_b[:], in_=ident_f[:]), ["ident_f"], ["ident_b"])
    dve(lambda e: e.tensor_copy(out=wg_b[:], in_=wg_f[:]), ["wg_f"], ["wg_b"])
    dve(lambda e: e.tensor_copy(out=poolw_b[:], in_=poolw_f[:]), ["poolw_f"], ["poolw_b"])
    dve(lambda e: e.memset(ones_f[:], 1.0 / 128.0), [], ["ones_f"])
    dve(lambda e: e.memset(ones_b[:], 1.0), [], ["ones_b"])
    dve(lambda e: e.memset(epsc[:], EPS), [], ["epsc"])
    dve(lambda e: e.memset(gT[:], 1.0), [], ["gT"])

    for j in range(NPAN):
        S.dma("pool", "cv%d" % (j % NCV),
              lambda e, j=j: e.dma_start(out=wbf_d.ap()[j], in_=wpan_d.ap()[j]),
              writes=[("wbf", j)])

    seq_panels = []
    for s in range(nseq):
        seq_panels += ["kv%d" % i for i in range(4)]
        for t in range(ntile):
            seq_panels += TILE_PANELS
    pstate = {"next_load": 0, "next_use": 0}

    def panel_load():
        i = pstate["next_load"]
        if i >= len(seq_panels):
            return
        pstate["next_load"] = i + 1
        slot = i % RING
        pid = PID[seq_panels[i]]
        m_ = S.mute
        S.mute = False
        S.dma("sp", "ring%d" % slot,
              lambda e: e.dma_start(out=ring[slot][:].rearrange("p a b -> p (a b)"), in_=wbf_d.ap()[pid]),
              reads=[("wbf", pid)], writes=[("ring", slot)])
        S.mute = m_

    def panel_next(expect):
        i = pstate["next_use"]
        assert seq_panels[i] == expect, (seq_panels[i], expect)
        pstate["next_use"] = i + 1
        slot = i % RING
        return ring[slot], ("ring", slot)

    def initial_ring_fill():
        for _ in range(RING):
            panel_load()

    def norm_T(xap_of_block, xkeys_of_block, nblk, widx):
        ntok = nblk * 128
        for b in range(nblk):
            xin = xap_of_block(b)
            act(lambda e, b=b, xin=xin: e.activation(out=junk[:], in_=xin, func=AF.Square,
                                                     accum_out=ss[:, b:b + 1]),
                xkeys_of_block(b), ["junk", ("ss", b)])
        sskeys = [("ss", b) for b in range(nblk)]
        act(lambda e: e.activation(out=rstd[:, 0:nblk], in_=ss[:, 0:nblk], func=AF.Ln,
                                   scale=1.0 / D, bias=epsc[:, 0:1]), sskeys + ["epsc"], [("rstd", b_) for b_ in range(4)])
        act(lambda e: e.activation(out=rstd[:, 0:nblk], in_=rstd[:, 0:nblk], func=AF.Exp, scale=-0.5),
            [("rstd", b_) for b_ in range(4)], [("rstd", b_) for b_ in range(4)])
        for b in range(nblk):
            xin = xap_of_block(b)
            dve(lambda e, b=b, xin=xin: e.tensor_scalar(out=hb[:, b, :], in0=xin,
                                                        scalar1=rstd[:, b:b + 1], scalar2=None, op0=ALU.mult),
                xkeys_of_block(b) + [("rstd", b)], [("hb", b)])
        for kc in range(KC):
            pt, ptk = npt()

            def fn(e, kc=kc, pt=pt):
                ins = None
                for b in range(nblk):
                    ins = e.transpose(out=pt[:, b * 128:(b + 1) * 128],
                                      in_=hb[:, b, kc * 128:(kc + 1) * 128], identity=ident_b[:])
                return ins
            S.op("pe", fn, [("hb", b) for b in range(nblk)] + ["ident_b"], [ptk])
            if kc % 2 == 0:
                act(lambda e, kc=kc, pt=pt: e.activation(out=hT[:, kc, 0:ntok], in_=pt[:, 0:ntok], func=AF.Copy,
                                                         scale=nw[:, widx, kc:kc + 1]),
                    [ptk, "nw"], [("hT", kc)])
            else:
                dve(lambda e, kc=kc, pt=pt: e.tensor_scalar(out=hT[:, kc, 0:ntok], in0=pt[:, 0:ntok],
                                                            scalar1=nw[:, widx, kc:kc + 1], scalar2=None,
                                                            op0=ALU.mult),
                    [ptk, "nw"], [("hT", kc)])

    def norm_x(xb_, xk_, widx):
        for b in range(NB):
            xin = xb_[:, b, :]
            act(lambda e, b=b, xin=xin: e.activation(out=junk[:], in_=xin, func=AF.Square,
                                                     accum_out=ss[:, b:b + 1]), [(xk_, b)], ["junk", ("ss", b)])
            act(lambda e, b=b: e.activation(out=rstd[:, b:b + 1], in_=ss[:, b:b + 1], func=AF.Ln,
                                            scale=1.0 / D, bias=epsc[:, 0:1]), [("ss", b), "epsc"], [("rstd", b)])
            act(lambda e, b=b: e.activation(out=rstd[:, b:b + 1], in_=rstd[:, b:b + 1], func=AF.Exp, scale=-0.5),
                [("rstd", b)], [("rstd", b)])
            dve(lambda e, b=b, xin=xin: e.tensor_scalar(out=hb[:, b, :], in0=xin, scalar1=rstd[:, b:b + 1],
                                                        scalar2=None, op0=ALU.mult),
                [(xk_, b), ("rstd", b)], [("hb", b)])
            pt, ptk = npt()

            def fn(e, b=b, pt=pt):
                ins = None
                for kc in range(KC):
                    ins = e.transpose(out=pt[:, kc * 128:(kc + 1) * 128],
                                      in_=hb[:, b, kc * 128:(kc + 1) * 128], identity=ident_b[:])
                return ins
            S.op("pe", fn, [("hb", b), "ident_b"], [ptk])
            dve(lambda e, b=b, pt=pt: e.tensor_tensor(
                out=hT[:, :, b * 128:(b + 1) * 128],
                in0=pt[:, :].rearrange("p (k t) -> p k t", k=KC),
                in1=nw[:, widx, :].unsqueeze(2).broadcast_to([128, KC, 128]), op=ALU.mult),
                [ptk, "nw"], [("hT", kc) for kc in range(KC)])

    hTk = [("hT", kc) for kc in range(KC)]

    def proj_residual(src, names, xb, xk):
        pans = [panel_next(nm) for nm in names]
        for b in range(NB):
            for nh, (pan, pk) in enumerate(pans):
                bank, bk = nb()
                mm(bank[:, :], [(src.ap[:, kc, b * 128:(b + 1) * 128], pan[:, kc, :]) for kc in range(KC)],
                   src.k() + [pk], [bk])
                dve(lambda e, b=b, nh=nh, bank=bank: e.tensor_tensor(
                    out=xb[:, b, nh * 512:(nh + 1) * 512], in0=xb[:, b, nh * 512:(nh + 1) * 512],
                    in1=bank[:, :], op=ALU.add), [bk, (xk, b)], [(xk, b)])
        for _ in names:
            panel_load()

    def load_mem(s_):
        S.dma("sp", "mem", lambda e: e.dma_start(
            out=m_sb[:], in_=mem_d.ap()[s_ * NMEM:(s_ + 1) * NMEM, :].rearrange("(b p) d -> p b d", p=128)),
            writes=["m_sb"])

    def load_x(s_, t_):
        g = s_ * ntile + t_
        r0 = s_ * SEQ + t_ * T
        S.dma("sp", "x%d" % (g % 2), lambda e: e.dma_start(
            out=xbuf[g % 2][:], in_=x_d.ap()[r0:r0 + T, :].rearrange("(b p) d -> p b d", p=128)),
            writes=[("x%d" % (g % 2), b) for b in range(NB)])

    tile_idx = 0
    for s in range(nseq):
        if s == 0:
            load_mem(0)
            load_x(0, 0)
            initial_ring_fill()
        S.mute = en is not None and "kv" not in en
        norm_T(lambda b: m_sb[:, b, :], lambda b: ["m_sb"], 2, 2)
        for i in range(2):
            pan, pk = panel_next("kv%d" % i)
            for c in range(4):
                bank, bk = nb()
                mm(bank[:, 0:NMEM], [(pan[:, kc, c * 128:(c + 1) * 128], hT[:, kc, 0:NMEM]) for kc in range(KC)],
                   hTk + [pk], [bk])
                act(lambda e, i=i, c=c, bank=bank: e.activation(out=KT[:, i * 4 + c, :], in_=bank[:, 0:NMEM],
                                                                func=AF.Copy), [bk], [("KT", i * 4 + c)])
            panel_load()
        for i in range(2):
            pan, pk = panel_next("kv%d" % (2 + i))
            for mb in range(2):
                bank, bk = nb()
                mm(bank[:, :], [(hT[:, kc, mb * 128:(mb + 1) * 128], pan[:, kc, :]) for kc in range(KC)],
                   hTk + [pk], [bk])
                act(lambda e, i=i, mb=mb, bank=bank: e.activation(out=Vm[:, mb, i * 512:(i + 1) * 512],
                                                                  in_=bank[:, :], func=AF.Copy),
                    [bk], [("Vm", mb)])
            panel_load()
        S.mute = False
        dve(lambda e: e.memset(Sst[:], 0.0), [], ["Sst"])
        dve(lambda e: e.memset(pext[:, :, 0:16], 0.0), [], ["pext_h"])
        dve(lambda e: e.memset(halo[:], 0.0), [], ["halo"] + [("halo", q) for q in range(NCH)])

        for t in range(ntile):
            xb = xbuf[tile_idx % 2]
            xk = "x%d" % (tile_idx % 2)
            row0 = s * SEQ + t * T
            xkeys = [(xk, b) for b in range(NB)]
            if t + 1 < ntile:
                load_x(s, t + 1)
            elif s + 1 < nseq:
                load_x(s + 1, 0)
                load_mem(s + 1)

            S.mute = en is not None and "mix" not in en
            norm_x(xb, xk, 0)
            bank, bk = nb()
            mm(bank[0:16, :], [(wg_b[:, kc, :], hT[:, kc, :]) for kc in range(KC)], hTk + ["wg_b"], [bk])
            act(lambda e, bank=bank: e.activation(out=gT[0:16, :], in_=bank[0:16, :], func=AF.Copy), [bk], ["gT"])
            for b in range(NB):
                if b % 2 == 0:
                    zb, zk = nb()
                zo = zb[:, (b % 2) * 256:(b % 2 + 1) * 256]
                mm(zo, [(gT[0:17, b * 128:(b + 1) * 128], gkw[0:17, :])], ["gT", "gkw"], [zk])
                act(lambda e, b=b, zo=zo: e.activation(out=lg.ap[:, b, :], in_=zo, func=AF.Exp, scale=-1.0),
                    [zk], lg.kc(b))
                act(lambda e, b=b: e.activation(out=lg.ap[:, b, :], in_=lg.ap[:, b, :], func=AF.Ln, bias=1.0),
                    lg.kc(b), lg.kc(b))
            ck(1)
            pan, pk = panel_next("in_p")
            for c in range(4):
                bank, bk = nb()
                mm(bank[:, :], [(pan[:, kc, c * 128:(c + 1) * 128], hT[:, kc, :]) for kc in range(KC)],
                   hTk + [pk], [bk])
                act(lambda e, c=c, bank=bank: e.activation(out=pext[:, c, 16:16 + T], in_=bank[:, :], func=AF.Copy),
                    [bk], [("pext", c)])
            panel_load()
            ck(2)
            GT = []
            for pr in range(2):
                bank, bk = nb()
                mm_multi([(bank[:, b * 128:(b + 1) * 128], [(lg.ap[:, b, pr * 128:(pr + 1) * 128], tri[:, :])])
                          for b in range(NB)], lg.k() + ["tri"], [bk])
                GT.append((bank, bk))
                act(lambda e, pr=pr, bank=bank: e.activation(out=eG.ap[:, pr, :], in_=bank[:, :], func=AF.Exp),
                    [bk], eG.kc(pr))
                act(lambda e, pr=pr, bank=bank: e.activation(out=enG.ap[:, pr, :], in_=bank[:, :], func=AF.Exp,
                                                             scale=-1.0), [bk], enG.kc(pr))
                act(lambda e, pr=pr, bank=bank: e.activation(
                    out=dec[:, pr, :], in_=bank[:, :].rearrange("p (c j) -> p c j", j=64)[:, :, 63], func=AF.Exp),
                    [bk], [("dec", pr)])
            eDk = []
            for b in range(NB):
                if b % 2 == 0:
                    db, dk_ = nb()
                do = db[:, (b % 2) * 256:(b % 2 + 1) * 256]
                mm(do, [(tris[:, :], lg.ap[:, b, :])], lg.kc(b) + ["tris"], [dk_])
                act(lambda e, do=do, b=b: e.activation(out=eD.ap[:, b, :], in_=do, func=AF.Exp), [dk_], eD.kc(b))
            ck(3)
            pan, pk = panel_next("in_qk")
            for c in range(4):
                bank, bk = nb()
                mm(bank[:, :], [(pan[:, kc, c * 128:(c + 1) * 128], hT[:, kc, :]) for kc in range(KC)],
                   hTk + [pk], [bk])
                if c < 2:
                    dve(lambda e, c=c, bank=bank: e.scalar_tensor_tensor(
                        out=qdec.ap[:, c, :], in0=bank[:, :], scalar=0.125, in1=eG.ap[:, c, :],
                        op0=ALU.mult, op1=ALU.mult), [bk] + eG.kc(c), qdec.kc(c))
                else:
                    dve(lambda e, c=c, bank=bank: e.tensor_tensor(
                        out=kdec.ap[:, c - 2, :], in0=bank[:, :], in1=enG.ap[:, c - 2, :], op=ALU.mult),
                        [bk] + enG.kc(c - 2), kdec.kc(c - 2))
            for b in range(NB):
                if b % 2 == 0:
                    kb_, kk_ = nb()
                ko = kb_[:, (b % 2) * 256:(b % 2 + 1) * 256]
                mm(ko, [(hT[:, kc, b * 128:(b + 1) * 128], pan[:, kc, 256:512]) for kc in range(KC)],
                   hTk + [pk], [kk_])
                dve(lambda e, b=b, ko=ko: e.tensor_tensor(out=kte.ap[:, b, :], in0=ko, in1=eD.ap[:, b, :],
                                                          op=ALU.mult), [kk_] + eD.kc(b), kte.kc(b))
            panel_load()
            ck(4)
            pan, pk = panel_next("in_v")
            for b in range(NB):
                bank, bk = nb()
                mm(bank[:, :], [(hT[:, kc, b * 128:(b + 1) * 128], pan[:, kc, :]) for kc in range(KC)],
                   hTk + [pk], [bk])
                act(lambda e, b=b, bank=bank: e.activation(out=vtok.ap[:, b, :], in_=bank[:, :], func=AF.Copy),
                    [bk], vtok.kc(b))
            panel_load()
            ck(5)
            pextk = [("pext", c) for c in range(4)]
            for g in range(4):
                src_ap = pext[:, g, :]
                src_k = [("pext", g), "pext_h"]
                L = 16 + T
                sh = 1
                for it in range(g + 1):
                    dst = pa[it % 2]
                    dve(lambda e, src_ap=src_ap, dst=dst, sh=sh, L=L: e.tensor_tensor(
                        out=dst.ap[:, 2 * sh - 1:L], in0=src_ap[:, 2 * sh - 1:L], in1=src_ap[:, sh - 1:L - sh],
                        op=ALU.add),
                        src_k, dst.k())
                    src_ap = dst.ap
                    src_k = dst.k()
                    sh *= 2
                w = 2 ** (g + 1)
                dve(lambda e, g=g, src_ap=src_ap, w=w: e.scalar_tensor_tensor(
                    out=pooled.ap[:, g, :], in0=src_ap[:, 16:16 + T], scalar=1.0 / w, in1=pext[:, g, 16:16 + T],
                    op0=ALU.mult, op1=ALU.subtract), src_k + [("pext", g)], pooled.kc(g))
                if t == 0:
                    dve(lambda e, g=g, src_ap=src_ap: e.tensor_tensor(
                        out=src_ap[:, 16:32], in0=src_ap[:, 16:32], in1=invc[:, g, :], op=ALU.mult),
                        src_k + ["invc"], src_k)
                    dve(lambda e, g=g, src_ap=src_ap: e.tensor_tensor(
                        out=pooled.ap[:, g, 0:16], in0=src_ap[:, 16:32], in1=pext[:, g, 16:32], op=ALU.subtract),
                        src_k + [("pext", g)], pooled.kc(g))
                bank, bk = nb()
                mm(bank[:, :], [(poolw_b[:, g, :], pooled.ap[:, g, :])], pooled.kc(g) + ["poolw_b"], [bk])
                act(lambda e, g=g, bank=bank: e.activation(out=catT.ap[:, g, :], in_=bank[:, :], func=AF.Copy,
                                                           scale=pscale[:, g:g + 1]), [bk, "pscale"], catT.kc(g))
            dve(lambda e: e.tensor_copy(out=pext[:, :, 0:16], in_=pext[:, :, T:T + 16]), pextk + ["pext_h"],
                ["pext_h"])
            ck(7)
            for hh in range(2):
                for hl in range(2):
                    scb, sck = nb()
                    groups = []
                    for bl in range(2):
                        b = 2 * hh + bl
                        for hf in range(2):
                            c = 2 * b + hf
                            for hp in range(2):
                                groups.append((scb[64 * hf:64 * hf + 64, bl * 128 + hp * 64: bl * 128 + hp * 64 + 64],
                                               [(kdec.ap[64 * hl:64 * hl + 64, hp, c * 64:(c + 1) * 64],
                                                 qdec.ap[64 * hl:64 * hl + 64, hp, c * 64:(c + 1) * 64])]))
                    mm_multi(groups, kdec.k() + qdec.k(), [sck])
                    dve(lambda e, hh=hh, hl=hl, scb=scb: e.tensor_tensor(
                        out=scm.ap[:, 2 * hh:2 * hh + 2, hl * 128:(hl + 1) * 128],
                        in0=scb[:, 0:256].rearrange("p (a b) -> p a b", a=2),
                        in1=cmask2[:, :, :],
                        op=ALU.mult), [sck, "cmask"], scm.kc(2 * hh, 2 * hh + 2))
                ck(8)
                kvb = []
                for hf in range(2):
                    bank, bk = nb()
                    groups = []
                    for bl in range(2):
                        b = 2 * hh + bl
                        for h in range(4):
                            pr, hl = divmod(h, 2)
                            groups.append((bank[64 * hl:64 * hl + 64, bl * 256 + pr * 128: bl * 256 + pr * 128 + 128],
                                           [(kte.ap[64 * hf:64 * hf + 64, b, h * 64:(h + 1) * 64],
                                             vtok.ap[64 * hf:64 * hf + 64, b, h * 128:(h + 1) * 128])]))
                    mm_multi(groups, kte.kc(2 * hh, 2 * hh + 2) + vtok.kc(2 * hh, 2 * hh + 2), [bk])
                    kvb.append((bank, bk))
                ck(9)
                for cl in range(4):
                    c = 4 * hh + cl
                    bl, hf = divmod(cl, 2)
                    bank, bk = kvb[hf]
                    act(lambda e, c=c: e.activation(out=Sbf.ap[:, c, :], in_=Sst[:].rearrange("p a b -> p (a b)"),
                                                    func=AF.Copy), ["Sst"], Sbf.kc(c))
                    for pr in range(2):
                        dve(lambda e, c=c, pr=pr, bank=bank, bl=bl: e.scalar_tensor_tensor(
                            out=Sst[:, pr, :], in0=Sst[:, pr, :], scalar=dec[:, pr, c:c + 1],
                            in1=bank[:, bl * 256 + pr * 128: bl * 256 + pr * 128 + 128],
                            op0=ALU.mult, op1=ALU.add), ["Sst", bk, ("dec", pr)], ["Sst"])
            ck(6)
            pan, pk = panel_next("in_r")
            for c in range(4):
                bank, bk = nb()
                mm(bank[:, :], [(pan[:, kc, c * 128:(c + 1) * 128], hT[:, kc, :]) for kc in range(KC)],
                   hTk + [pk], [bk])
                act(lambda e, c=c, bank=bank: e.activation(out=sr.ap[:, c, :], in_=bank[:, :], func=AF.Silu),
                    [bk], sr.kc(c))
            panel_load()
            for hh in range(2):
                ck(10)
                oib = []
                for hf in range(2):
                    bank, bk = nb()
                    groups = []
                    for bl in range(2):
                        b = 2 * hh + bl
                        for h in range(4):
                            hp, hl = divmod(h, 2)
                            groups.append((bank[:, bl * 256 + h * 64: bl * 256 + h * 64 + 64],
                                           [(vtok.ap[64 * hf:64 * hf + 64, b, h * 128:(h + 1) * 128],
                                             scm.ap[64 * hf:64 * hf + 64, b, (hl * 2 + hp) * 64:(hl * 2 + hp) * 64 + 64])]))
                    mm_multi(groups, vtok.kc(2 * hh, 2 * hh + 2) + scm.kc(2 * hh, 2 * hh + 2), [bk])
                    oib.append((bank, bk))
                ck(11)
                for hl in range(2):
                    bank, bk = nb()
                    groups = []
                    for cl in range(4):
                        c = 4 * hh + cl
                        for hp in range(2):
                            groups.append((bank[:, cl * 128 + hp * 64: cl * 128 + hp * 64 + 64],
                                           [(Sbf.ap[64 * hl:64 * hl + 64, c, hp * 128:(hp + 1) * 128],
                                             qdec.ap[64 * hl:64 * hl + 64, hp, c * 64:(c + 1) * 64])]))
                    mm_multi(groups, Sbf.kc(4 * hh, 4 * hh + 4) + qdec.k(), [bk])
                    for hp in range(2):
                        act(lambda e, hl=hl, hp=hp, hh=hh, bank=bank: e.activation(
                            out=o_sb.ap[:, 2 * hp + hl, hh * 256:(hh + 1) * 256].rearrange("p (c i) -> p c i", i=64),
                            in_=bank[:, :].rearrange("p (c q i) -> p c q i", q=2, i=64)[:, :, hp, :],
                            func=AF.Copy), [bk], o_sb.kc(2 * hp + hl))
                ck(12)
                for hf in range(2):
                    bank, bk = oib[hf]
                    for bl in range(2):
                        cl = 2 * bl + hf
                        dve(lambda e, hh=hh, bl=bl, cl=cl, bank=bank: e.tensor_tensor(
                            out=o_sb.ap[:, :, hh * 256 + cl * 64: hh * 256 + cl * 64 + 64],
                            in0=o_sb.ap[:, :, hh * 256 + cl * 64: hh * 256 + cl * 64 + 64],
                            in1=bank[:, bl * 256:(bl + 1) * 256].rearrange("p (h i) -> p h i", i=64),
                            op=ALU.add), [bk] + o_sb.k(), o_sb.k())
            for h in range(4):
                act(lambda e, h=h: e.activation(out=sq.ap[:, h, :], in_=o_sb.ap[:, h, :], func=AF.Square),
                    o_sb.kc(h), sq.kc(h))
            ck(13)
            for h in range(4):
                bank, bk = nb()
                mm(bank[:, :], [(ones_f[:, :], sq.ap[:, h, :])], sq.kc(h) + ["ones_f"], [bk])
                r_ = rb[h % 2]
                act(lambda e, bank=bank, r_=r_: e.activation(out=r_.ap, in_=bank[:, :], func=AF.Ln,
                                                             bias=epsc[:, 0:1]), [bk, "epsc"], r_.k())
                act(lambda e, r_=r_: e.activation(out=r_.ap, in_=r_.ap, func=AF.Exp, scale=-0.5), r_.k(), r_.k())
                dve(lambda e, h=h, r_=r_: e.scalar_tensor_tensor(
                    out=gl.ap, in0=o_sb.ap[:, h, :], scalar=gnw[:, 0:1], in1=r_.ap, op0=ALU.mult, op1=ALU.mult),
                    o_sb.kc(h) + r_.k() + ["gnw"], gl.k())
                dve(lambda e, h=h: e.tensor_tensor(out=catT.ap[:, 4 + h, :], in0=gl.ap, in1=sr.ap[:, h, :],
                                                   op=ALU.mult), gl.k() + sr.kc(h), catT.kc(4 + h))
            ck(14)
            proj_residual(catT, ["wout0", "wout1"], xb, xk)

            S.mute = en is not None and "xat" not in en
            norm_x(xb, xk, 1)
            for i in range(2):
                pan, pk = panel_next("wq%d" % i)
                for c in range(4):
                    bank, bk = nb()
                    mm(bank[:, :], [(pan[:, kc, c * 128:(c + 1) * 128], hT[:, kc, :]) for kc in range(KC)],
                       hTk + [pk], [bk])
                    cc = i * 4 + c
                    if c % 2 == 0:
                        act(lambda e, cc=cc, bank=bank: e.activation(out=qxT.ap[:, cc, :], in_=bank[:, :],
                                                                     func=AF.Copy), [bk], qxT.kc(cc))
                    else:
                        dve(lambda e, cc=cc, bank=bank: e.tensor_copy(out=qxT.ap[:, cc, :], in_=bank[:, :]),
                            [bk], qxT.kc(cc))
                panel_load()
            for h in range(4):
                for mc in range(2):
                    bank, bk = nb()
                    mm(bank[:, :], [(KT[:, 2 * h + dc, mc * 128:(mc + 1) * 128], qxT.ap[:, 2 * h + dc, :])
                                    for dc in range(2)],
                       qxT.kc(2 * h, 2 * h + 2) + [("KT", 2 * h), ("KT", 2 * h + 1)], [bk])
                    act(lambda e, h=h, mc=mc, bank=bank: e.activation(out=PT.ap[:, 2 * h + mc, :], in_=bank[:, :],
                                                                      func=AF.Exp, scale=1.0 / 16.0),
                        [bk], PT.kc(2 * h + mc))
                smb, smk = nb()
                mm(smb[:, :], [(ones_b[:, :], PT.ap[:, 2 * h + mc, :]) for mc in range(2)],
                   PT.kc(2 * h, 2 * h + 2) + ["ones_b"], [smk])
                r_ = rs[h % 2]
                act(lambda e, smb=smb, r_=r_: e.activation(out=r_.ap, in_=smb[:, :], func=AF.Ln), [smk], r_.k())
                act(lambda e, r_=r_: e.activation(out=r_.ap, in_=r_.ap, func=AF.Exp, scale=-1.0), r_.k(), r_.k())
                for dc in range(2):
                    bank, bk = nb()
                    mm(bank[:, :], [(Vm[:, mc, h * 256 + dc * 128: h * 256 + dc * 128 + 128], PT.ap[:, 2 * h + mc, :])
                                    for mc in range(2)],
                       PT.kc(2 * h, 2 * h + 2) + [("Vm", 0), ("Vm", 1)], [bk])
                    dve(lambda e, h=h, dc=dc, bank=bank, r_=r_: e.tensor_tensor(
                        out=oxT.ap[:, 2 * h + dc, :], in0=bank[:, :], in1=r_.ap, op=ALU.mult),
                        [bk] + r_.k(), oxT.kc(2 * h + dc))
            proj_residual(oxT, ["wo0", "wo1"], xb, xk)

            S.mute = en is not None and "ffn" not in en
            norm_x(xb, xk, 3)
            for j in range(11):
                pan, pk = panel_next("up%d" % j)
                ys = []
                for r4 in range(4):
                    q = 4 * j + r4
                    bank, bk = nb()
                    mm(bank[:, :], [(pan[:, kc, r4 * 128:(r4 + 1) * 128], hT[:, kc, :]) for kc in range(KC)],
                       hTk + [pk], [bk])
                    ub = ubuf[q % 4]
                    yb = ybuf[q % 4]
                    act(lambda e, q=q, ub=ub: e.activation(out=ub.ap[:, 0:2], in_=halo[:, q, :], func=AF.Copy),
                        [("halo", q), "halo"], ub.k())
                    act(lambda e, ub=ub, bank=bank: e.activation(out=ub.ap[:, 2:2 + T], in_=bank[:, :], func=AF.Copy),
                        [bk], ub.k())
                    act(lambda e, q=q, yb=yb, bank=bank: e.activation(
                        out=yb.ap, in_=bank[:, :], func=AF.Identity, scale=cw[:, q, 2:3], bias=cb[:, q:q + 1]),
                        [bk, "cw", "cb"], yb.k())
                    act(lambda e, q=q, bank=bank: e.activation(out=halo[:, q, :], in_=bank[:, T - 2:T], func=AF.Copy),
                        [bk], [("halo", q)])
                    dve(lambda e, q=q, ub=ub, yb=yb: e.scalar_tensor_tensor(
                        out=yb.ap, in0=ub.ap[:, 1:1 + T], scalar=cw[:, q, 1:2], in1=yb.ap,
                        op0=ALU.mult, op1=ALU.add), ub.k() + yb.k() + ["cw"], yb.k())
                    dve(lambda e, q=q, ub=ub, yb=yb: e.scalar_tensor_tensor(
                        out=yb.ap, in0=ub.ap[:, 0:T], scalar=cw[:, q, 0:1], in1=yb.ap,
                        op0=ALU.mult, op1=ALU.add), ub.k() + yb.k() + ["cw"], yb.k())
                    ys.append(yb)
                panel_load()
                for r2 in range(2):
                    ch = 2 * j + r2
                    sgb = sg[ch % 2]
                    yg, yv = ys[r2], ys[2 + r2]
                    act(lambda e, yg=yg, sgb=sgb: e.activation(out=sgb.ap, in_=yg.ap, func=AF.Silu),
                        yg.k(), sgb.k())
                    dve(lambda e, ch=ch, sgb=sgb, yv=yv: e.tensor_tensor(out=actT.ap[:, ch, :], in0=sgb.ap,
                                                                         in1=yv.ap, op=ALU.mult),
                        sgb.k() + yv.k(), actT.kc(ch))
            for nh in range(2):
                banks = [nb() for _ in range(NB)]
                for kg in range(3):
                    pan, pk = panel_next("dn%d_%d" % (nh, kg))
                    nk = 8 if kg < 2 else 6
                    for b in range(NB):
                        bank, bk = banks[b]

                        def fn(e, b=b, bank=bank, kg=kg, nk=nk, pan=pan):
                            ins = None
                            for kc in range(nk):
                                ins = e.matmul(bank[:, :], actT.ap[:, kg * 8 + kc, b * 128:(b + 1) * 128],
                                               pan[:, kc, :], start=(kg == 0 and kc == 0),
                                               stop=(kg == 2 and kc == nk - 1))
                            return ins
                        S.op("pe", fn, actT.kc(kg * 8, kg * 8 + nk) + [pk], [bk])
                    panel_load()
                for b in range(NB):
                    bank, bk = banks[b]
                    dve(lambda e, b=b, nh=nh, bank=bank, xb=xb: e.tensor_tensor(
                        out=xb[:, b, nh * 512:(nh + 1) * 512], in0=xb[:, b, nh * 512:(nh + 1) * 512],
                        in1=bank[:, :], op=ALU.add), [bk, (xk, b)], [(xk, b)])

            S.mute = en is not None and "fin" not in en
            for b in range(NB):
                act(lambda e, b=b, xb=xb: e.activation(out=junk[:], in_=xb[:, b, :], func=AF.Square,
                                                accum_out=ss[:, b:b + 1]), [(xk, b)], ["junk", ("ss", b)])
            act(lambda e: e.activation(out=rstd[:, :], in_=ss[:, :], func=AF.Ln, scale=1.0 / D, bias=epsc[:, 0:1]),
                [("ss", b) for b in range(NB)] + ["epsc"], [("rstd", b_) for b_ in range(4)])
            act(lambda e: e.activation(out=rstd[:, :], in_=rstd[:, :], func=AF.Exp, scale=-0.5),
                [("rstd", b_) for b_ in range(4)], [("rstd", b_) for b_ in range(4)])
            for b in range(NB):
                dve(lambda e, b=b, xb=xb: e.scalar_tensor_tensor(out=xb[:, b, :], in0=xb[:, b, :], scalar=rstd[:, b:b + 1],
                                                          in1=wf_bc[:, :], op0=ALU.mult, op1=ALU.mult),
                    [(xk, b), ("rstd", b), "wf_bc"], [(xk, b)])
            S.mute = False
            S.dma("sp", xk, lambda e, xb=xb, row0=row0: e.dma_start(
                out=out_d.ap()[row0:row0 + T, :].rearrange("(b p) d -> p b d", p=128), in_=xb[:]),
                reads=xkeys, writes=[("out", tile_idx)])
            tile_idx += 1

    S.wait_all("sp", [S.lastw[("out", i)] for i in range(tile_idx)])
    S.emit()
    return nc


def _prep_shared(inp):
    f = lambda a: np.ascontiguousarray(np.asarray(a, dtype=np.float32))
    w_in = f(inp["w_in"])[0]
    tri, tris, cmask, ident, invc = _consts()
    col = lambda v, n: np.ascontiguousarray(f(v).reshape(n, 128).T)
    nw = np.stack([col(inp["norm_mix_w"][0], 8), col(inp["norm_xattn_w"][0], 8),
                   col(inp["norm_mem_w"][0], 8), col(inp["norm_ffn_w"][0], 8)], axis=1)
    wg = w_in[:, 1536:1552].reshape(8, 128, 16).transpose(1, 0, 2)
    poolw = f(inp["pool_w"])[0].transpose(1, 0, 2)
    gkw = np.concatenate([f(inp["gk_w2"])[0], f(inp["gk_b"])[0][None, :]], axis=0)
    qcols = np.array([_chunk_col(q) for q in range(NCH)])
    idx = qcols[:, None] + np.arange(128)[None, :]
    cwf = f(inp["ffn_conv_w"])[0]
    cw = cwf[:, idx].transpose(2, 1, 0)
    cb = f(inp["ffn_conv_b"])[0][idx].T
    shared = {
        "wpan": _pack_weights(w_in, f(inp["w_out"])[0], f(inp["xattn_wq"])[0], f(inp["xattn_wkv"])[0],
                              f(inp["xattn_wo"])[0], f(inp["ffn_w_up"])[0], f(inp["ffn_w_down"])[0]),
        "wg": np.ascontiguousarray(wg).reshape(128, 128),
        "nw": np.ascontiguousarray(nw).reshape(128, 32),
        "wf": f(inp["norm_final_w"]).reshape(1, D),
        "poolw": np.ascontiguousarray(poolw).reshape(128, 512),
        "pscale": col(inp["pool_scale"][0], 4),
        "gkw": np.ascontiguousarray(gkw),
        "gnw": f(inp["gla_norm_w"])[0].reshape(128, 1),
        "cw": np.ascontiguousarray(cw).reshape(128, NCH * 3),
        "cb": np.ascontiguousarray(cb),
        "tri": tri, "tris": tris, "cmask": cmask, "ident": ident, "invc": invc,
    }
    return shared


def kernel(**inputs):
    x = np.asarray(inputs["x"], dtype=np.float32)
    mem = np.asarray(inputs["mem"], dtype=np.float32)
    shared = _prep_shared(inputs)
    nc = build_program()
    in_maps = []
    for c in range(NCORES):
        m = dict(shared)
        m["x"] = np.ascontiguousarray(x[c * SEQ_PER_CORE:(c + 1) * SEQ_PER_CORE]).reshape(SEQ_PER_CORE * SEQ, D)
        m["mem"] = np.ascontiguousarray(mem[c * SEQ_PER_CORE:(c + 1) * SEQ_PER_CORE]).reshape(SEQ_PER_CORE * NMEM, D)
        in_maps.append(m)
    res = run_bass_kernel_spmd(nc, in_maps, core_ids=list(range(NCORES)))
    out = np.concatenate([r["out"].reshape(SEQ_PER_CORE, SEQ, D) for r in res.results], axis=0)
    return out.astype(np.float32)
```
